# Optimizing a Trainium2 kernel written in Bass

```python
import math
import jax
import jax.numpy as jnp
from jax import lax
import numpy as np

D_MODEL = 4096
BATCH = 2
SEQ = 4096
DEPTH = 2

CTX_LEN = 256
GRID_W = 64
ROPE_BASE = 10000.0
LN_EPS = 1e-5
DEEPNORM_ALPHA = (2 * DEPTH) ** 0.25
DEEPNORM_BETA = (8 * DEPTH) ** -0.25

D_RNN = D_MODEL // 2
RNN_BLOCKS = 16
RNN_BLOCK = D_RNN // RNN_BLOCKS
CONV_W = 4
CONV_LEFT = (CONV_W - 1) // 2
LRU_C = 8.0

WA_HEAD_DIM = 128
WA_HEADS = D_MODEL // 256
WA_KV_HEADS = WA_HEADS // 4
WINDOW = 128
BLOCK = 128

DA_HEAD_DIM = 64
DA_HEADS = D_MODEL // 256
DA_V_DIM = 2 * DA_HEAD_DIM
Q_BLOCK = 128

N_EXPERTS = 32
TOP_K = 4
D_EXPERT = D_MODEL // 8
SWIGLU_LIMIT = 7.0
SWIGLU_ALPHA = 1.702

N_BRANCHES = 3
IN_SIZES = (D_RNN, WA_HEADS * WA_HEAD_DIM, WA_KV_HEADS * WA_HEAD_DIM, WA_KV_HEADS * WA_HEAD_DIM,
            DA_HEADS * 2 * DA_HEAD_DIM, DA_HEADS * 2 * DA_HEAD_DIM, DA_HEADS * DA_V_DIM,
            N_BRANCHES * D_MODEL)
D_IN = sum(IN_SIZES)

kernel_name = 'hybrid_rglru_swa_diffattn_moe_dit'


def layer_norm(x):
    xf = x.astype(jnp.float32)
    mu = jnp.mean(xf, -1, keepdims=True)
    var = jnp.mean(jnp.square(xf - mu), -1, keepdims=True)
    return ((xf - mu) * lax.rsqrt(var + LN_EPS)).astype(x.dtype)


def layer_norm_affine(x, g, b):
    return (layer_norm(x) * g + b).astype(x.dtype)


def modulate(h, shift, scale):
    return h * (1 + scale) + shift


def lambda_init(layer):
    return 0.8 - 0.6 * math.exp(-0.3 * layer)


def split_columns(t, sizes):
    outs, start = [], 0
    for n in sizes:
        outs.append(t[..., start:start + n])
        start += n
    return outs


def axial_rope(pos_row, pos_col, head_dim):
    n = head_dim // 4
    inv = ROPE_BASE ** (-jnp.arange(n, dtype=jnp.float32) / n)
    ang = jnp.concatenate([pos_row[:, None].astype(jnp.float32) * inv,
                           pos_col[:, None].astype(jnp.float32) * inv], -1)
    return jnp.cos(ang), jnp.sin(ang)


def apply_rope(t, cos, sin):
    t1, t2 = jnp.split(t, 2, axis=-1)
    cs = cos[None, :, None, :].astype(t.dtype)
    sn = sin[None, :, None, :].astype(t.dtype)
    return jnp.concatenate([t1 * cs - t2 * sn, t1 * sn + t2 * cs], -1)


def depthwise_conv(u, w, b):
    L = u.shape[1]
    up = jnp.pad(u, ((0, 0), (CONV_LEFT, CONV_W - 1 - CONV_LEFT), (0, 0)))
    return b + sum(w[k] * up[:, k:k + L] for k in range(CONV_W))


def rglru_coeffs(v, w_r, b_r, w_i, b_i, lam):
    B, L, _ = v.shape
    vb = v.reshape(B, L, RNN_BLOCKS, RNN_BLOCK)
    r = jax.nn.sigmoid(jnp.einsum('blnd,nde->blne', vb, w_r.astype(jnp.float32)).reshape(B, L, D_RNN) + b_r)
    i = jax.nn.sigmoid(jnp.einsum('blnd,nde->blne', vb, w_i.astype(jnp.float32)).reshape(B, L, D_RNN) + b_i)
    log_a = -LRU_C * r * jax.nn.softplus(-lam.astype(jnp.float32))
    return jnp.exp(log_a), jnp.sqrt(-jnp.expm1(2.0 * log_a)) * (i * v)


def _scan_op(e1, e2):
    a1, b1 = e1
    a2, b2 = e2
    return a1 * a2, a2 * b1 + b2


def linear_scan(a, b, h0, reverse):
    t = -1 if reverse else 0
    b = b.at[:, t].add(a[:, t] * h0)
    return lax.associative_scan(_scan_op, (a, b), reverse=reverse, axis=1)[1]


def rglru_branch(ux, uc, p, with_ctx):
    vx = depthwise_conv(ux, p['conv_w'], p['conv_b']).astype(jnp.float32)
    vc = depthwise_conv(uc, p['conv_w'], p['conv_b']).astype(jnp.float32)
    h0 = jnp.zeros((ux.shape[0], D_RNN), jnp.float32)
    hx = 0.0
    ctx_scans = []
    for d, reverse in enumerate((False, True)):
        gates = (p['lru_wr'][d], p['lru_br'][d], p['lru_wi'][d], p['lru_bi'][d], p['lru_lambda'][d])
        h_ctx = linear_scan(*rglru_coeffs(vc, *gates), h0, reverse)
        h_final = h_ctx[:, 0] if reverse else h_ctx[:, -1]
        hx = hx + linear_scan(*rglru_coeffs(vx, *gates), h_final, reverse)
        ctx_scans.append(h_ctx)
    yc = (ctx_scans[0] + ctx_scans[1]).astype(uc.dtype) if with_ctx else None
    return hx.astype(ux.dtype), yc


def band_blocks(t):
    B, S, H, d = t.shape
    tb = t.reshape(B, S // BLOCK, BLOCK, H, d)
    tp = jnp.pad(tb, ((0, 0), (1, 1), (0, 0), (0, 0), (0, 0)))
    return jnp.concatenate([tp[:, :-2], tp[:, 1:-1], tp[:, 2:]], axis=2)


def window_gqa(q, k, v, qc, kc, vc, sink, with_ctx):
    B, S, _, dh = q.shape
    C = kc.shape[1]
    nb = S // BLOCK
    G = WA_HEADS // WA_KV_HEADS
    nk = 3 * BLOCK
    scale = dh ** -0.5
    sink_f = sink.astype(jnp.float32).reshape(WA_KV_HEADS, G)
    qb = q.reshape(B, nb, BLOCK, WA_KV_HEADS, G, dh)
    kb, vb = band_blocks(k), band_blocks(v)
    s_loc = jnp.einsum('bnqhgd,bnkhd->bnhgqk', qb, kb).astype(jnp.float32) * scale
    s_ctx = jnp.einsum('bnqhgd,bkhd->bnhgqk', qb, kc).astype(jnp.float32) * scale
    qpos = jnp.arange(S).reshape(nb, BLOCK)
    kpos = (jnp.arange(nb)[:, None] - 1) * BLOCK + jnp.arange(nk)[None, :]
    kp = kpos[:, None, :]
    valid = (kp >= 0) & (kp < S) & (jnp.abs(kp - qpos[:, :, None]) <= WINDOW)
    s_loc = jnp.where(valid[None, :, None, None], s_loc, -jnp.inf)
    s_sink = jnp.broadcast_to(sink_f[None, None, :, :, None, None], s_loc.shape[:-1] + (1,))
    pr = jax.nn.softmax(jnp.concatenate([s_loc, s_ctx, s_sink], -1), axis=-1).astype(v.dtype)
    out = (jnp.einsum('bnhgqk,bnkhd->bnqhgd', pr[..., :nk], vb)
           + jnp.einsum('bnhgqk,bkhd->bnqhgd', pr[..., nk:nk + C], vc))
    yx = out.reshape(B, S, WA_HEADS * dh)
    yc = None
    if with_ctx:
        qcg = qc.reshape(B, C, WA_KV_HEADS, G, dh)
        s = jnp.einsum('bqhgd,bkhd->bhgqk', qcg, kc).astype(jnp.float32) * scale
        s_sink_c = jnp.broadcast_to(sink_f[None, :, :, None, None], s.shape[:-1] + (1,))
        pc = jax.nn.softmax(jnp.concatenate([s, s_sink_c], -1), axis=-1).astype(vc.dtype)
        yc = jnp.einsum('bhgqk,bkhd->bqhgd', pc[..., :C], vc).reshape(B, C, WA_HEADS * dh)
    return yx, yc


def diff_attention(q, k, v, qc, kc, vc, p, lam_init, with_ctx):
    B, S, H, _, dh = q.shape
    scale = dh ** -0.5
    lam = (jnp.exp(jnp.sum(p['da_lq1'].astype(jnp.float32) * p['da_lk1']))
           - jnp.exp(jnp.sum(p['da_lq2'].astype(jnp.float32) * p['da_lk2'])) + lam_init)

    def attend(qb, kk, vv):
        s = jnp.einsum('bqhmd,bkhmd->bmhqk', qb, kk).astype(jnp.float32) * scale
        a = jax.nn.softmax(s, axis=-1)
        w = (a[:, 0] - lam * a[:, 1]).astype(vv.dtype)
        return jnp.einsum('bhqk,bkhd->bqhd', w, vv)

    def sub_norm(o):
        of = o.astype(jnp.float32)
        of = of * lax.rsqrt(jnp.mean(jnp.square(of), -1, keepdims=True) + LN_EPS) * p['da_norm_g'] * (1.0 - lam_init)
        return of.reshape(o.shape[0], o.shape[1], H * DA_V_DIM).astype(o.dtype)

    k_all = jnp.concatenate([k, kc], axis=1)
    v_all = jnp.concatenate([v, vc], axis=1)
    nb = S // Q_BLOCK
    q_blocks = q.reshape(B, nb, Q_BLOCK, H, 2, dh).swapaxes(0, 1)
    ox = lax.map(lambda qb: attend(qb, k_all, v_all), q_blocks)
    ox = ox.swapaxes(0, 1).reshape(B, S, H, DA_V_DIM)
    yc = sub_norm(attend(qc, kc, vc)) if with_ctx else None
    return sub_norm(ox), yc


def merge_branches(ya, yb, yc, g, p):
    ga, gb, gc = jnp.split(jax.nn.sigmoid(g), N_BRANCHES, axis=-1)
    m = ga * (ya @ p['w_br_a']) + gb * (yb @ p['w_br_b']) + gc * (yc @ p['w_br_c'])
    return m @ p['w_out'] + p['b_out']


def token_mixing(hx, hc, p, rope_wa, rope_da, lam_init, with_ctx):
    B, S, _ = hx.shape
    C = hc.shape[1]
    ua_x, qa_x, ka_x, va_x, qd_x, kd_x, vd_x, g_x = split_columns(hx @ p['w_in'], IN_SIZES)
    ua_c, qa_c, ka_c, va_c, qd_c, kd_c, vd_c, g_c = split_columns(hc @ p['w_in'], IN_SIZES)
    ya_x, ya_c = rglru_branch(ua_x, ua_c, p, with_ctx)
    yb_x, yb_c = window_gqa(
        apply_rope(qa_x.reshape(B, S, WA_HEADS, WA_HEAD_DIM), *rope_wa),
        apply_rope(ka_x.reshape(B, S, WA_KV_HEADS, WA_HEAD_DIM), *rope_wa),
        va_x.reshape(B, S, WA_KV_HEADS, WA_HEAD_DIM),
        qa_c.reshape(B, C, WA_HEADS, WA_HEAD_DIM),
        ka_c.reshape(B, C, WA_KV_HEADS, WA_HEAD_DIM),
        va_c.reshape(B, C, WA_KV_HEADS, WA_HEAD_DIM),
        p['wa_sink'], with_ctx)
    qd = apply_rope(qd_x.reshape(B, S, 2 * DA_HEADS, DA_HEAD_DIM), *rope_da).reshape(B, S, DA_HEADS, 2, DA_HEAD_DIM)
    kd = apply_rope(kd_x.reshape(B, S, 2 * DA_HEADS, DA_HEAD_DIM), *rope_da).reshape(B, S, DA_HEADS, 2, DA_HEAD_DIM)
    yc_x, yc_c = diff_attention(
        qd, kd, vd_x.reshape(B, S, DA_HEADS, DA_V_DIM),
        qd_c.reshape(B, C, DA_HEADS, 2, DA_HEAD_DIM),
        kd_c.reshape(B, C, DA_HEADS, 2, DA_HEAD_DIM),
        vd_c.reshape(B, C, DA_HEADS, DA_V_DIM),
        p, lam_init, with_ctx)
    mx = merge_branches(ya_x, yb_x, yc_x, g_x, p)
    mc = merge_branches(ya_c, yb_c, yc_c, g_c, p) if with_ctx else None
    return mx, mc


def moe_ffn(h, q):
    logits = (h @ q['router_w'] + q['router_b']).astype(jnp.float32)
    top_v, top_i = lax.top_k(logits, TOP_K)
    w = jax.nn.softmax(top_v, axis=-1)
    combine = jnp.einsum('tk,tke->te', w, jax.nn.one_hot(top_i, N_EXPERTS, dtype=jnp.float32)).astype(h.dtype)
    out = jnp.zeros_like(h)
    for e in range(N_EXPERTS):
        gate = jnp.minimum(h @ q['w_gate'][e] + q['b_gate'][e], SWIGLU_LIMIT)
        up = jnp.clip(h @ q['w_up'][e] + q['b_up'][e], -SWIGLU_LIMIT, SWIGLU_LIMIT)
        y = ((up + 1) * gate * jax.nn.sigmoid(SWIGLU_ALPHA * gate)) @ q['w_down'][e] + q['b_down'][e]
        out = out + combine[:, e:e + 1] * y
    return out


def setup_inputs(seed: int = 0) -> dict:
    key = jax.random.key(seed)
    keys = jax.random.split(key, 40)
    f32 = jnp.float32
    L, D, E, F = DEPTH, D_MODEL, N_EXPERTS, D_EXPERT
    wa_w = WA_HEADS * WA_HEAD_DIM
    da_w = DA_HEADS * DA_V_DIM

    def nrm(i, shape, scale):
        return jax.random.normal(keys[i], shape, f32) * scale

    a0 = jax.random.uniform(keys[13], (L, 2, D_RNN), f32, 0.9, 0.999) ** (1.0 / LRU_C)
    return {
        'x': nrm(0, (BATCH, SEQ, D), 1.0),
        'c': nrm(1, (BATCH, D), 1.0),
        'ctx': nrm(2, (BATCH, CTX_LEN, D), 1.0),
        'c_ctx': nrm(3, (D,), 1.0),
        'w_ada': nrm(4, (L, D, 6 * D), 0.5 * D ** -0.5),
        'b_ada': nrm(5, (L, 6 * D), 0.01),
        'w_in': nrm(6, (L, D, D_IN), D ** -0.5),
        'conv_w': nrm(7, (L, CONV_W, D_RNN), CONV_W ** -0.5),
        'conv_b': nrm(8, (L, D_RNN), 0.01),
        'lru_wr': nrm(9, (L, 2, RNN_BLOCKS, RNN_BLOCK, RNN_BLOCK), RNN_BLOCK ** -0.5),
        'lru_br': nrm(10, (L, 2, D_RNN), 0.01),
        'lru_wi': nrm(11, (L, 2, RNN_BLOCKS, RNN_BLOCK, RNN_BLOCK), RNN_BLOCK ** -0.5),
        'lru_bi': nrm(12, (L, 2, D_RNN), 0.01),
        'lru_lambda': jnp.log(a0) - jnp.log1p(-a0),
        'wa_sink': nrm(14, (L, WA_HEADS), 0.5),
        'da_lq1': nrm(15, (L, DA_HEAD_DIM), 0.1),
        'da_lk1': nrm(16, (L, DA_HEAD_DIM), 0.1),
        'da_lq2': nrm(17, (L, DA_HEAD_DIM), 0.1),
        'da_lk2': nrm(18, (L, DA_HEAD_DIM), 0.1),
        'da_norm_g': 1.0 + nrm(19, (L, DA_V_DIM), 0.01),
        'w_br_a': nrm(20, (L, D_RNN, D), DEEPNORM_BETA * D_RNN ** -0.5),
        'w_br_b': nrm(21, (L, wa_w, D), DEEPNORM_BETA * wa_w ** -0.5),
        'w_br_c': nrm(22, (L, da_w, D), DEEPNORM_BETA * da_w ** -0.5),
        'w_out': nrm(23, (L, D, D), DEEPNORM_BETA * D ** -0.5),
        'b_out': nrm(24, (L, D), 0.01),
        'ln1_g': 1.0 + nrm(25, (L, D), 0.01),
        'ln1_b': nrm(26, (L, D), 0.01),
        'router_w': nrm(27, (L, D, E), D ** -0.5),
        'router_b': nrm(28, (L, E), 0.01),
        'exp_w_gate': nrm(29, (L, E, D, F), D ** -0.5),
        'exp_b_gate': nrm(30, (L, E, F), 0.01),
        'exp_w_up': nrm(31, (L, E, D, F), D ** -0.5),
        'exp_b_up': nrm(32, (L, E, F), 0.01),
        'exp_w_down': nrm(33, (L, E, F, D), DEEPNORM_BETA * F ** -0.5),
        'exp_b_down': nrm(34, (L, E, D), 0.01),
        'ln2_g': 1.0 + nrm(35, (L, D), 0.01),
        'ln2_b': nrm(36, (L, D), 0.01),
    }


def reference(x, c, ctx, c_ctx, w_ada, b_ada, w_in, conv_w, conv_b, lru_wr, lru_br, lru_wi, lru_bi,
              lru_lambda, wa_sink, da_lq1, da_lk1, da_lq2, da_lk2, da_norm_g, w_br_a, w_br_b, w_br_c,
              w_out, b_out, ln1_g, ln1_b, router_w, router_b, exp_w_gate, exp_b_gate, exp_w_up, exp_b_up,
              exp_w_down, exp_b_down, ln2_g, ln2_b):
    B, S, D = x.shape
    C = ctx.shape[1]
    rows = S // GRID_W
    pos_row = jnp.repeat(jnp.arange(rows, dtype=jnp.int32), GRID_W)
    pos_col = jnp.tile(jnp.arange(GRID_W, dtype=jnp.int32), rows)
    rope_wa = axial_rope(pos_row, pos_col, WA_HEAD_DIM)
    rope_da = axial_rope(pos_row, pos_col, DA_HEAD_DIM)
    silu_c = jax.nn.silu(c)
    silu_cc = jax.nn.silu(c_ctx)
    for l in range(DEPTH):
        with_ctx = l < DEPTH - 1
        p = {'w_in': w_in[l], 'conv_w': conv_w[l], 'conv_b': conv_b[l],
             'lru_wr': lru_wr[l], 'lru_br': lru_br[l], 'lru_wi': lru_wi[l], 'lru_bi': lru_bi[l],
             'lru_lambda': lru_lambda[l], 'wa_sink': wa_sink[l],
             'da_lq1': da_lq1[l], 'da_lk1': da_lk1[l], 'da_lq2': da_lq2[l], 'da_lk2': da_lk2[l],
             'da_norm_g': da_norm_g[l], 'w_br_a': w_br_a[l], 'w_br_b': w_br_b[l], 'w_br_c': w_br_c[l],
             'w_out': w_out[l], 'b_out': b_out[l]}
        q = {'router_w': router_w[l], 'router_b': router_b[l],
             'w_gate': exp_w_gate[l], 'b_gate': exp_b_gate[l], 'w_up': exp_w_up[l], 'b_up': exp_b_up[l],
             'w_down': exp_w_down[l], 'b_down': exp_b_down[l]}
        mod_x = (silu_c @ w_ada[l] + b_ada[l])[:, None, :]
        mod_c = silu_cc @ w_ada[l] + b_ada[l]
        sh1x, sc1x, g1x, sh2x, sc2x, g2x = jnp.split(mod_x, 6, axis=-1)
        sh1c, sc1c, g1c, sh2c, sc2c, g2c = jnp.split(mod_c, 6, axis=-1)
        mx, mc = token_mixing(modulate(layer_norm(x), sh1x, sc1x), modulate(layer_norm(ctx), sh1c, sc1c),
                              p, rope_wa, rope_da, lambda_init(l), with_ctx)
        x = layer_norm_affine(DEEPNORM_ALPHA * x + g1x * mx, ln1_g[l], ln1_b[l])
        hx = modulate(layer_norm(x), sh2x, sc2x).reshape(B * S, D)
        if with_ctx:
            ctx = layer_norm_affine(DEEPNORM_ALPHA * ctx + g1c * mc, ln1_g[l], ln1_b[l])
            hc = modulate(layer_norm(ctx), sh2c, sc2c).reshape(B * C, D)
            f = moe_ffn(jnp.concatenate([hx, hc], axis=0), q)
            fx = f[:B * S]
            ctx = layer_norm_affine(DEEPNORM_ALPHA * ctx + g2c * f[B * S:].reshape(B, C, D), ln2_g[l], ln2_b[l])
        else:
            fx = moe_ffn(hx, q)
        x = layer_norm_affine(DEEPNORM_ALPHA * x + g2x * fx.reshape(B, S, D), ln2_g[l], ln2_b[l])
    return x
```

```python
import contextlib
import numpy as np
import ml_dtypes
import concourse.bass as bass
import concourse.mybir as mybir
import concourse.bass_utils as _bu

F32, BF16 = mybir.dt.float32, mybir.dt.bfloat16
AF = mybir.ActivationFunctionType
ALU = mybir.AluOpType
BF = ml_dtypes.bfloat16

D = 4096
T = 1088
NL = 1024
NCX = 64
TB = 4352
D_IN = 23552
ALPHA = 4 ** 0.25
LN_EPS = 1e-5
CHUNKS = [(0, 512), (512, 512), (1024, 64)]
TTILES = [(i * 128, 128) for i in range(8)] + [(1024, 64)]

ENGS = ("tensor", "vector", "scalar", "gpsimd", "sync")


class Sched:
    def __init__(self, nc):
        self.nc = nc
        self.ops = []
        self.last_w = {}
        self.readers = {}

    def op(self, eng, fn, reads=(), writes=(), dma=None):
        i = len(self.ops)
        deps = set()
        for r in reads:
            w = self.last_w.get(r)
            if w is not None:
                deps.add(w)
        for w_ in writes:
            w = self.last_w.get(w_)
            if w is not None:
                deps.add(w)
            for rd in self.readers.get(w_, ()):
                deps.add(rd)
        deps.discard(i)
        if eng == "tensor":
            deps = {d for d in deps if self.ops[d]["eng"] != "tensor"}
        self.ops.append(dict(eng=eng, fn=fn, deps=deps, dma=dma, sig=False))
        for r in reads:
            self.readers.setdefault(r, []).append(i)
        for w_ in writes:
            self.last_w[w_] = i
            self.readers[w_] = []
        return i

    PH = 0

    def emit(self):
        Sched.PH += 1
        nc = self.nc
        ops = self.ops
        for o in ops:
            for d in o["deps"]:
                ops[d]["sig"] = True
        for o in ops:
            if o["dma"] is not None:
                o["sig"] = True
        for eng in ENGS:
            my = [o for o in ops if o["eng"] == eng and o["dma"] is None]
            if my:
                my[-1]["sig"] = True
        counts = {}
        epoch_ctr = {}
        for o in ops:
            if not o["sig"]:
                continue
            if o["dma"] is not None:
                k = ("dma", o["dma"])
                inc = 16
            else:
                ep = epoch_ctr.get(o["eng"], 0) // 20000
                epoch_ctr[o["eng"]] = epoch_ctr.get(o["eng"], 0) + 1
                k = ("eng", o["eng"], ep)
                inc = 1
            counts[k] = counts.get(k, 0) + inc
            o["semk"] = k
            o["semv"] = counts[k]
            o["inc"] = inc
        with contextlib.ExitStack() as es:
            sems = {}
            for k in counts:
                sems[k] = es.enter_context(nc.semaphore("s%d_" % Sched.PH + "_".join(str(x) for x in k)))
            block = es.enter_context(nc.Block())
            for eng in ENGS:
                my = [o for o in ops if o["eng"] == eng]

                def body(e, my=my, eng=eng):
                    seen = {}
                    for o in my:
                        need = {}
                        for d in o["deps"]:
                            od = ops[d]
                            k, v = od["semk"], od["semv"]
                            if need.get(k, 0) < v:
                                need[k] = v
                        for k, v in need.items():
                            if seen.get(k, 0) >= v:
                                continue
                            e.wait_ge(sems[k], v)
                            seen[k] = v
                        ins = o["fn"](e)
                        if o["sig"]:
                            ins.then_inc(sems[o["semk"]], o["inc"])
                    for k, v in counts.items():
                        if seen.get(k, 0) < v:
                            e.wait_ge(sems[k], v)

                getattr(block, eng)(body)


class KB:
    def __init__(self):
        KB.NID = 0
        Sched.PH = 0
        self.nc = bass.Bass("TRN2", target_bir_lowering=False)
        self.begin()

    def begin(self):
        self.s = Sched(self.nc)
        self.es = contextlib.ExitStack()

    def end(self):
        self.s.emit()
        self.es.close()

    def din(self, name, shape, dt=F32):
        return self.nc.dram_tensor(name, list(shape), dt, kind="ExternalInput").ap()

    def dout(self, name, shape, dt=F32):
        return self.nc.dram_tensor(name, list(shape), dt, kind="ExternalOutput").ap()

    NID = 0

    def sb(self, name, shape, dt=F32):
        KB.NID += 1
        return self.es.enter_context(self.nc.sbuf_tensor("%s_u%d" % (name, KB.NID), list(shape), dt))

    def ps(self, name, shape, dt=F32):
        KB.NID += 1
        return self.es.enter_context(self.nc.psum_tensor("%s_u%d" % (name, KB.NID), list(shape), dt))

    def op(self, *a, **k):
        return self.s.op(*a, **k)

    def finish(self):
        self.end()
        return self.nc


def rev(ap, n):
    return bass.AP(ap.tensor, ap.offset + n - 1, [ap.ap[0], [-1, n]])


def build_pm():
    kb = KB()
    nc = kb.nc
    cT = kb.din("cT", [128, 96])
    wada = kb.din("w_ada", [2 * D, 3072])
    bada = kb.din("b_adaT", [128, 48])
    mod_o = kb.dout("modS", [128, 144])
    pan = [kb.sb("pan%d" % i, [128, 32, 512], BF16) for i in range(2)]
    cTs = kb.sb("cTs", [128, 96]); sT = kb.sb("sT", [128, 96], BF16)
    bsb = kb.sb("bsb", [128, 48]); msb = kb.sb("msb", [128, 144])
    psm = kb.ps("psm", [128, 512])
    op = kb.op
    op("sync", lambda e: e.dma_start(out=cTs[:], in_=cT), writes=["cTs"], dma="c0")
    op("sync", lambda e: e.dma_start(out=bsb[:], in_=bada), writes=["bsb"], dma="c1")
    op("scalar", lambda e: e.activation(out=sT[:], in_=cTs[:], func=AF.Silu), reads=["cTs"], writes=["sT"])
    sT3 = sT[:].rearrange("p (k r) -> p k r", r=3)
    wv = wada.rearrange("(l kt p) n -> l p kt n", l=2, p=128)
    for pi in range(12):
        l, pj = divmod(pi, 6)
        sl = pi % 2
        for q in range(4):
            op("gpsimd", lambda e, sl=sl, q=q, l=l, pj=pj: e.dma_start(out=pan[sl][:, q * 8:(q + 1) * 8, :], in_=wv[l, :, q * 8:(q + 1) * 8, pj * 512:(pj + 1) * 512]),
               writes=["pan%d_%d" % (sl, q)], dma="pan%d" % sl)

        def mm(e, sl=sl, pi=pi):
            ins = None
            for ct in range(4):
                idx = pi * 4 + ct
                for kt in range(32):
                    ins = e.matmul(psm[:, idx * 3:idx * 3 + 3], lhsT=pan[sl][:, kt, ct * 128:(ct + 1) * 128], rhs=sT3[:, kt, :], start=(kt == 0), stop=(kt == 31))
            return ins
        op("tensor", mm, reads=["pan%d_%d" % (sl, q_) for q_ in range(4)] + ["sT"], writes=["psm"])
    m3 = msb[:].rearrange("p (k r) -> p k r", r=3)
    p3 = psm[:, 0:144].rearrange("p (k r) -> p k r", r=3)
    for r in range(3):
        op("vector", lambda e, r=r: e.tensor_tensor(out=m3[:, :, r], in0=p3[:, :, r], in1=bsb[:], op=ALU.add), reads=["psm", "bsb"], writes=["msb"])
    op("sync", lambda e: e.dma_start(out=mod_o, in_=msb[:]), reads=["msb"], writes=["mod_o"], dma="mo")
    return kb.finish()


def build_p1(nblk=2):
    kb = KB()
    kb.end()
    nc = kb.nc
    x_a = kb.din("x", [nblk * T, D])
    modT_d = kb.din("modT", [128, 384])
    win = kb.din("w_in", [D, D_IN])
    tabs_a = kb.din("rope", [nblk * 4, 128, T])
    psw_d = kb.din("psw", [2, 128, 128], BF16)
    ident_d = kb.din("ident", [128, 128])
    FM_a = kb.dout("FM", [nblk * 20992, T], BF16)
    VA_a = kb.dout("VA", [nblk * T, 512], BF16)
    VD_a = kb.dout("VD", [nblk * T, 2048], BF16)
    for blk in range(nblk):
        p1_body(kb, x_a[blk * T:(blk + 1) * T, :], modT_d, win, tabs_a[blk * 4:(blk + 1) * 4], psw_d, ident_d,
                FM_a[blk * 20992:(blk + 1) * 20992, :], VA_a[blk * T:(blk + 1) * T, :], VD_a[blk * T:(blk + 1) * T, :])
    return kb.nc


def p1_body(kb, x, modT_d, win, tabs_d, psw_d, ident_d, FM_o, VA_o, VD_o):
    kb.begin()
    hT = kb.sb("hT", [128, 32, T], BF16)
    pan = [kb.sb("pan%d" % i, [128, 32, 512], BF16) for i in range(2)]
    tabs = kb.sb("tabs", [128, 4, T])
    psw = kb.sb("psw_sb", [128, 2, 128], BF16)
    ident = kb.sb("ident_sb", [128, 128])
    modT = kb.sb("modT_sb", [128, 384])
    ops1 = kb.sb("ops1", [128, 64])
    xt = kb.sb("xt", [128, D])
    xn = kb.sb("xn", [128, D])
    stats = kb.sb("stats", [128, 48])
    mv = kb.sb("mv", [128, 4])
    stg = [kb.sb("stg%d" % i, [128, T], BF16) for i in range(2)]
    stv = [kb.sb("stv%d" % i, [128, 512], BF16) for i in range(2)]
    qb = [kb.sb("qb%d" % i, [128, 512], BF16) for i in range(2)]
    t1 = [kb.sb("t1_%d" % i, [128, 512]) for i in range(2)]
    t2 = [kb.sb("t2_%d" % i, [128, 512]) for i in range(2)]

    pst = [kb.ps("pst%d" % i, [128, 512]) for i in range(2)]
    psp = [kb.ps("psp%d" % i, [128, 512]) for i in range(3)]
    psr = [kb.ps("psr%d" % i, [128, 512]) for i in range(2)]

    op = kb.op
    op("sync", lambda e: e.dma_start(out=modT[:], in_=modT_d), writes=["modT"], dma="c0")
    op("sync", lambda e: e.dma_start(out=ident[:], in_=ident_d), writes=["ident"], dma="c2")
    op("sync", lambda e: e.dma_start(out=tabs[:], in_=tabs_d.rearrange("a p t -> p a t")), writes=["tabs"], dma="c3")
    op("sync", lambda e: e.dma_start(out=psw[:], in_=psw_d.rearrange("a p t -> p a t")), writes=["psw"], dma="c4")
    modT3 = modT[:].rearrange("p (k r) -> p k r", r=2)
    op("vector", lambda e: e.tensor_scalar(out=ops1[:], in0=modT[:, 64:128], scalar1=1.0, scalar2=None, op0=ALU.add),
       reads=["modT"], writes=["ops1"])
    ops13 = ops1[:].rearrange("p (k r) -> p k r", r=2)

    for tt, (t0, n) in enumerate(TTILES):
        r = 0 if tt < 8 else 1
        op("sync", lambda e, t0=t0, n=n: e.dma_start(out=xt[:n, :], in_=x[t0:t0 + n, :]), writes=["xt"], dma="xt")

        def st(e, n=n):
            ins = None
            for c in range(8):
                ins = e.bn_stats(out=stats[:n, c * 6:(c + 1) * 6], in_=xt[:n, c * 512:(c + 1) * 512])
            return ins
        op("vector", st, reads=["xt"], writes=["stats"])
        op("vector", lambda e, n=n: e.bn_aggr(out=mv[:n, 0:2], in_=stats[:n, :]), reads=["stats"], writes=["mv"])
        op("vector", lambda e, n=n: e.tensor_scalar(out=mv[:n, 2:3], in0=mv[:n, 1:2], scalar1=LN_EPS, scalar2=None,
                                                    op0=ALU.add), reads=["mv"], writes=["rs"])
        op("scalar", lambda e, n=n: e.activation(out=mv[:n, 3:4], in_=mv[:n, 2:3], func=AF.Sqrt), reads=["rs"], writes=["sd"])
        op("vector", lambda e, n=n: e.reciprocal(out=mv[:n, 2:3], in_=mv[:n, 3:4]), reads=["sd"], writes=["rs"])
        op("vector", lambda e, n=n: e.tensor_scalar(out=xn[:n, :], in0=xt[:n, :], scalar1=mv[:n, 0:1], scalar2=mv[:n, 2:3],
                                                    op0=ALU.subtract, op1=ALU.mult), reads=["xt", "mv", "rs"], writes=["xn"])
        for g in range(8):
            ps_ = pst[g % 2]

            def tr(e, g=g, n=n, ps_=ps_):
                ins = None
                for j in range(4):
                    dt = g * 4 + j
                    ins = e.transpose(out=ps_[:, j * 128:j * 128 + n], in_=xn[:n, dt * 128:(dt + 1) * 128],
                                      identity=ident[:n, :n])
                return ins
            op("tensor", tr, reads=["xn", "ident"], writes=["pst%d" % (g % 2)])

            def ev(e, g=g, n=n, ps_=ps_, t0=t0, r=r):
                ins = None
                for j in range(4):
                    dt = g * 4 + j
                    ins = e.tensor_scalar(out=hT[:, dt, t0:t0 + n], in0=ps_[:, j * 128:j * 128 + n],
                                          scalar1=ops13[:, dt, r:r + 1], scalar2=modT3[:, dt, r:r + 1],
                                          op0=ALU.mult, op1=ALU.add)
                return ins
            op("vector", ev, reads=["pst%d" % (g % 2), "ops1", "modT"], writes=["hT"])

    win_v = win.rearrange("(kt p) n -> p kt n", p=128)
    def fm_row(col):
        if col < 4608:
            return col
        if col < 5120:
            return None
        if col < 9216:
            return col - 512
        if col < 11264:
            return None
        return col - 2560
    cnt = 0
    for pi in range(46):
        sl = pi % 2
        for q in range(4):
            op("gpsimd", lambda e, sl=sl, q=q, pi=pi: e.dma_start(
                out=pan[sl][:, q * 8:(q + 1) * 8, :], in_=win_v[:, q * 8:(q + 1) * 8, pi * 512:(pi + 1) * 512]),
               writes=["pan%d_%d" % (sl, q)], dma="pan%d" % sl)
        col0 = pi * 512
        if fm_row(col0) is None:
            for tt, (t0, n) in enumerate(TTILES):
                pp = psp[cnt % 3]
                pk = "psp%d" % (cnt % 3)

                def mmv(e, sl=sl, t0=t0, n=n, pp=pp):
                    ins = None
                    for kt in range(32):
                        ins = e.matmul(pp[:n, :], lhsT=hT[:, kt, t0:t0 + n], rhs=pan[sl][:, kt, :],
                                       start=(kt == 0), stop=(kt == 31))
                    return ins
                op("tensor", mmv, reads=["pan%d_%d" % (sl, q_) for q_ in range(4)] + ["hT"], writes=[pk])
                sv = cnt % 2
                op("scalar", lambda e, pp=pp, n=n, sv=sv: e.activation(out=stv[sv][:n, :], in_=pp[:n, :], func=AF.Copy),
                   reads=[pk], writes=["stv%d" % sv])
                if col0 < 5120:
                    dst = VA_o[t0:t0 + n, :]
                else:
                    c = col0 - 9216
                    dst = VD_o[t0:t0 + n, c:c + 512]
                op("sync", lambda e, dst=dst, sv=sv, n=n: e.dma_start(out=dst, in_=stv[sv][:n, :]),
                   reads=["stv%d" % sv], writes=["vout%d" % cnt], dma="stv%d" % sv)
                cnt += 1
            continue
        rope = None
        if 2048 <= col0 < 4608:
            rope = 0
        elif 5120 <= col0 < 9216:
            rope = 1
        for ct in range(4):
            row0 = fm_row(col0) + ct * 128
            sg = (pi * 4 + ct) % 2
            for (c0, n) in CHUNKS:
                pp = psp[cnt % 3]
                pk = "psp%d" % (cnt % 3)

                def mmf(e, sl=sl, ct=ct, c0=c0, n=n, pp=pp):
                    ins = None
                    for kt in range(32):
                        ins = e.matmul(pp[:, :n], lhsT=pan[sl][:, kt, ct * 128:(ct + 1) * 128], rhs=hT[:, kt, c0:c0 + n],
                                       start=(kt == 0), stop=(kt == 31))
                    return ins
                op("tensor", mmf, reads=["pan%d_%d" % (sl, q_) for q_ in range(4)] + ["hT"], writes=[pk])
                if rope is None:
                    if cnt % 2 == 0:
                        op("scalar", lambda e, pp=pp, n=n, sg=sg, c0=c0: e.activation(out=stg[sg][:, c0:c0 + n], in_=pp[:, :n], func=AF.Copy),
                           reads=[pk], writes=["stg%d" % sg])
                    else:
                        op("vector", lambda e, pp=pp, n=n, sg=sg, c0=c0: e.tensor_copy(out=stg[sg][:, c0:c0 + n], in_=pp[:, :n]),
                           reads=[pk], writes=["stg%d" % sg])
                else:
                    rb = cnt % 2
                    op("scalar", lambda e, pp=pp, n=n, rb=rb: e.activation(out=qb[rb][:, :n], in_=pp[:, :n], func=AF.Copy),
                       reads=[pk], writes=["qb%d" % rb])
                    op("tensor", lambda e, rb=rb, n=n, rope=rope: e.matmul(psr[rb][:, :n], lhsT=psw[:, rope, :], rhs=qb[rb][:, :n],
                                                                         start=True, stop=True),
                       reads=["qb%d" % rb, "psw"], writes=["psr%d" % rb])
                    op("vector", lambda e, rb=rb, n=n, c0=c0, rope=rope: e.tensor_tensor(
                        out=t1[rb][:, :n], in0=qb[rb][:, :n], in1=tabs[:, 2 * rope, c0:c0 + n], op=ALU.mult),
                       reads=["qb%d" % rb, "tabs"], writes=["t1_%d" % rb])
                    op("vector", lambda e, rb=rb, n=n, c0=c0, rope=rope: e.tensor_tensor(
                        out=t2[rb][:, :n], in0=psr[rb][:, :n], in1=tabs[:, 2 * rope + 1, c0:c0 + n], op=ALU.mult),
                       reads=["psr%d" % rb, "tabs"], writes=["t2_%d" % rb])
                    op("vector", lambda e, rb=rb, n=n, c0=c0, sg=sg: e.tensor_tensor(
                        out=stg[sg][:, c0:c0 + n], in0=t1[rb][:, :n], in1=t2[rb][:, :n], op=ALU.add),
                       reads=["t1_%d" % rb, "t2_%d" % rb], writes=["stg%d" % sg])
                cnt += 1
            op("sync", lambda e, row0=row0, sg=sg: e.dma_start(out=FM_o[row0:row0 + 128, :], in_=stg[sg][:, :]),
               reads=["stg%d" % sg], writes=["fm%d" % row0], dma="stg%d" % sg)
    kb.end()


def rope_tables(j):
    pos = np.arange(j * NL, (j + 1) * NL)
    prow = (pos // 64).astype(np.float32)
    pcol = (pos % 64).astype(np.float32)
    out = np.zeros((4, 128, T), np.float32)
    out[0, :, NL:] = 1.0
    out[2, :, NL:] = 1.0
    for ti, hd in ((0, 128), (1, 64)):
        n = hd // 4
        inv = (10000.0 ** (-np.arange(n, dtype=np.float32) / n)).astype(np.float32)
        ang = np.concatenate([prow[:, None] * inv, pcol[:, None] * inv], -1)
        cos, sin = np.cos(ang).T, np.sin(ang).T
        half = hd // 2
        for p in range(128):
            dd = p % hd
            if dd < half:
                out[2 * ti, p, :NL] = cos[dd]
                out[2 * ti + 1, p, :NL] = -sin[dd]
            else:
                out[2 * ti, p, :NL] = cos[dd - half]
                out[2 * ti + 1, p, :NL] = sin[dd - half]
    return out


def swap_perms():
    out = np.zeros((2, 128, 128), np.float32)
    for m in range(128):
        out[0, (m + 64) % 128, m] = 1.0
        k = m + 32 if (m % 64) < 32 else m - 32
        out[1, k, m] = 1.0
    return out.astype(BF)


import math


def lambda_init(layer):
    return 0.8 - 0.6 * math.exp(-0.3 * layer)


def p2_body(kb, layer, FMX, VX, Y_T, prm):
    nc = kb.nc
    op = kb.op
    LAT = 4096
    kb.begin()
    op = kb.op
    cw = kb.sb("cw", [128, 16]); cb = kb.sb("cb", [128, 4])
    lp = kb.sb("lp", [128, 24])
    cl = kb.sb("cl", [128, 8]); cl2 = kb.sb("cl2", [128, 8]); tmp8 = kb.sb("tmp8", [128, 8])
    ones = kb.sb("ones", [128, 1])
    wri = kb.sb("wri", [128, 16, 128], BF16)
    upad = kb.sb("upad", [128, LAT + 3], BF16); cpad = kb.sb("cpad", [128, 256 + 3], BF16)
    v32 = kb.sb("v32", [128, TB]); vb = kb.sb("vb", [128, TB], BF16)
    hf = kb.sb("hf", [128, TB]); hr = kb.sb("hr", [128, TB])
    yst = kb.sb("yst", [128, TB], BF16)
    tr_ = [kb.sb("tr%d" % i, [128, 512]) for i in range(2)]
    ti_ = [kb.sb("ti%d" % i, [128, 512]) for i in range(2)]
    ta_ = [kb.sb("ta%d" % i, [128, 512]) for i in range(2)]
    tg_ = [kb.sb("tg%d" % i, [128, 512]) for i in range(2)]
    tb_ = [kb.sb("tb%d" % i, [128, 512]) for i in range(2)]
    psr = [kb.ps("psr%d" % i, [128, 512]) for i in range(2)]
    psi = [kb.ps("psi%d" % i, [128, 512]) for i in range(2)]
    op("sync", lambda e: e.dma_start(out=cw[:], in_=prm["conv_w"]), writes=["cw"], dma="k0")
    op("sync", lambda e: e.dma_start(out=cb[:], in_=prm["conv_b"]), writes=["cb"], dma="k1")
    op("sync", lambda e: e.dma_start(out=lp[:], in_=prm["lru_p"]), writes=["lp"], dma="k2")
    op("gpsimd", lambda e: e.dma_start(out=wri[:], in_=prm["lru_w"].rearrange("a p e -> p a e")), writes=["wri"], dma="k3")
    op("vector", lambda e: e.memset(ones[:], 1.0), writes=["ones"])
    op("scalar", lambda e: e.activation(out=tmp8[:], in_=lp[:, 16:24], func=AF.Exp, scale=-1.0), reads=["lp"], writes=["tmp8"])
    op("scalar", lambda e: e.activation(out=cl[:], in_=tmp8[:], func=AF.Ln, bias=ones[:, 0:1]), reads=["tmp8", "ones"], writes=["cl0"])
    op("vector", lambda e: e.tensor_scalar(out=cl2[:], in0=cl[:], scalar1=-16.0, scalar2=None, op0=ALU.mult), reads=["cl0"], writes=["cl2"])
    op("vector", lambda e: e.tensor_scalar(out=cl[:], in0=cl[:], scalar1=-8.0, scalar2=None, op0=ALU.mult), reads=["cl0", "cl2"], writes=["cl"])
    for ct in range(4):
        op("vector", lambda e: e.memset(upad[:], 0.0), writes=["upad"])
        op("vector", lambda e: e.memset(cpad[:], 0.0), writes=["cpad"])
        op("sync", lambda e, ct=ct: e.dma_start(out=upad[:, 1:1 + LAT], in_=FMX[ct * 128:(ct + 1) * 128, 0:LAT]), writes=["upad"], dma="up")
        op("sync", lambda e, ct=ct: e.dma_start(out=cpad[:, 1:257], in_=FMX[ct * 128:(ct + 1) * 128, LAT:TB]), writes=["cpad"], dma="cp")
        for (src, L, o0, key) in ((upad, LAT, 0, "upad"), (cpad, 256, LAT, "cpad")):
            op("vector", lambda e, src=src, L=L, o0=o0, ct=ct: e.tensor_scalar(
                out=v32[:, o0:o0 + L], in0=src[:, 0:L], scalar1=cw[:, ct * 4:ct * 4 + 1], scalar2=cb[:, ct:ct + 1],
                op0=ALU.mult, op1=ALU.add), reads=[key, "cw", "cb"], writes=["v32"])
            for k in range(1, 4):
                op("vector", lambda e, src=src, L=L, o0=o0, ct=ct, k=k: e.scalar_tensor_tensor(
                    out=v32[:, o0:o0 + L], in0=src[:, k:k + L], scalar=cw[:, ct * 4 + k:ct * 4 + k + 1], in1=v32[:, o0:o0 + L],
                    op0=ALU.mult, op1=ALU.add), reads=[key, "cw", "v32"], writes=["v32"])
        op("scalar", lambda e: e.activation(out=vb[:], in_=v32[:], func=AF.Copy), reads=["v32"], writes=["vb"])
        cc = 0
        for d in range(2):
            H = hf if d == 0 else hr
            hk = "hf" if d == 0 else "hr"
            if d == 0:
                order = [(LAT, 256, None)] + [(k * 512, 512, None) for k in range(8)]
            else:
                order = [(LAT, 256, None)] + [(k * 512, 512, None) for k in range(7, -1, -1)]
            prev = None
            for (c0, n, _) in order:
                b_ = cc % 2
                cc += 1
                j = d * 4 + ct
                op("tensor", lambda e, b_=b_, c0=c0, n=n, j=j: e.matmul(psr[b_][:, :n], lhsT=wri[:, j, :], rhs=vb[:, c0:c0 + n], start=True, stop=True),
                   reads=["wri", "vb"], writes=["psr%d" % b_])
                op("tensor", lambda e, b_=b_, c0=c0, n=n, j=j: e.matmul(psi[b_][:, :n], lhsT=wri[:, 8 + j, :], rhs=vb[:, c0:c0 + n], start=True, stop=True),
                   reads=["wri", "vb"], writes=["psi%d" % b_])
                op("scalar", lambda e, b_=b_, n=n, j=j: e.activation(out=tr_[b_][:, :n], in_=psr[b_][:, :n], func=AF.Sigmoid, bias=lp[:, j:j + 1]),
                   reads=["psr%d" % b_, "lp"], writes=["tr%d" % b_])
                op("scalar", lambda e, b_=b_, n=n, j=j: e.activation(out=ti_[b_][:, :n], in_=psi[b_][:, :n], func=AF.Sigmoid, bias=lp[:, 8 + j:9 + j]),
                   reads=["psi%d" % b_, "lp"], writes=["ti%d" % b_])
                op("scalar", lambda e, b_=b_, n=n, j=j: e.activation(out=ta_[b_][:, :n], in_=tr_[b_][:, :n], func=AF.Exp, scale=cl[:, j:j + 1]),
                   reads=["tr%d" % b_, "cl"], writes=["ta%d" % b_])
                op("scalar", lambda e, b_=b_, n=n, j=j: e.activation(out=tg_[b_][:, :n], in_=tr_[b_][:, :n], func=AF.Exp, scale=cl2[:, j:j + 1]),
                   reads=["tr%d" % b_, "cl2"], writes=["tg%d" % b_])
                op("scalar", lambda e, b_=b_, n=n: e.activation(out=tg_[b_][:, :n], in_=tg_[b_][:, :n], func=AF.Sqrt, bias=ones[:, 0:1], scale=-1.0),
                   reads=["tg%d" % b_, "ones"], writes=["tg%d" % b_])
                op("vector", lambda e, b_=b_, n=n, c0=c0: e.tensor_tensor(out=tb_[b_][:, :n], in0=ti_[b_][:, :n], in1=v32[:, c0:c0 + n], op=ALU.mult),
                   reads=["ti%d" % b_, "v32"], writes=["tb%d" % b_])
                op("vector", lambda e, b_=b_, n=n: e.tensor_tensor(out=tb_[b_][:, :n], in0=tb_[b_][:, :n], in1=tg_[b_][:, :n], op=ALU.mult),
                   reads=["tb%d" % b_, "tg%d" % b_], writes=["tb%d" % b_])
                if d == 0:
                    init = 0.0 if prev is None else H[:, prev:prev + 1]
                    op("vector", lambda e, b_=b_, n=n, c0=c0, init=init, H=H: e.tensor_tensor_scan(
                        out=H[:, c0:c0 + n], data0=ta_[b_][:, :n], data1=tb_[b_][:, :n], initial=init, op0=ALU.mult, op1=ALU.add),
                       reads=["ta%d" % b_, "tb%d" % b_, hk], writes=[hk])
                    prev = c0 + n - 1
                else:
                    init = 0.0 if prev is None else H[:, prev:prev + 1]
                    op("vector", lambda e, b_=b_, n=n, c0=c0, init=init, H=H: e.tensor_tensor_scan(
                        out=rev(H[:, c0:c0 + n], n), data0=rev(ta_[b_][:, :n], n), data1=rev(tb_[b_][:, :n], n), initial=init,
                        op0=ALU.mult, op1=ALU.add), reads=["ta%d" % b_, "tb%d" % b_, hk], writes=[hk])
                    prev = c0
        op("vector", lambda e: e.tensor_tensor(out=yst[:], in0=hf[:], in1=hr[:], op=ALU.add), reads=["hf", "hr"], writes=["yst"])
        op("sync", lambda e, ct=ct: e.dma_start(out=Y_T[ct * 128:(ct + 1) * 128, :], in_=yst[:]), reads=["yst"], writes=["ya%d" % ct], dma="yst")
    kb.end()

    kb.begin()
    op = kb.op
    qT = kb.sb("qT", [128, 4, TB], BF16); kT = kb.sb("kT", [128, TB], BF16)
    v1 = kb.sb("v1", [128, 34, 129], BF16)
    ybT = kb.sb("ybT", [128, 4, TB], BF16)
    msk = kb.sb("msk", [128, 2, 128], BF16)
    identb = kb.sb("identb", [128, 128], BF16)
    esk = kb.sb("esk", [128, 4])
    E_ = [kb.sb("E%d" % i, [128, 512], BF16) for i in range(2)]
    den = kb.sb("den", [128, 8])
    ybq = [kb.sb("ybq%d" % i, [128, 128], BF16) for i in range(2)]
    pss = [kb.ps("pss%d" % i, [128, 512]) for i in range(2)]
    po = [kb.ps("po%d" % i, [128, 512]) for i in range(2)]
    pt = kb.ps("pt", [128, 1024], BF16)
    for h in range(4):
        op("sync", lambda e, h=h: e.dma_start(out=qT[:, h, :], in_=FMX[512 + h * 128:512 + (h + 1) * 128, :]), writes=["qT"], dma="q%d" % h)
    op("sync", lambda e: e.dma_start(out=kT[:], in_=FMX[2048:2176, :]), writes=["kT"], dma="kT")
    op("sync", lambda e: e.dma_start(out=v1[:, :, 0:128], in_=VX[:, 0:128].rearrange("(kt p) c -> p kt c", p=128)), writes=["v1"], dma="v1")
    op("vector", lambda e: e.memset(v1[:, :, 128:129], 1.0), writes=["v1o"])
    op("sync", lambda e: e.dma_start(out=msk[:], in_=prm["masks"].rearrange("a p t -> p a t")), writes=["msk"], dma="msk")
    op("sync", lambda e: e.dma_start(out=identb[:], in_=prm["identb"]), writes=["identb"], dma="idb")
    op("sync", lambda e: e.dma_start(out=esk[:], in_=prm["sink"]), writes=["esk"], dma="esk")
    op("scalar", lambda e: e.activation(out=esk[:], in_=esk[:], func=AF.Exp), reads=["esk"], writes=["esk"])
    scale = 128 ** -0.5
    cnt = 0
    tcnt = 0
    for qi in range(34):
        if qi < 32:
            kts = ([(qi - 1, 0)] if qi > 0 else []) + [(qi, None)] + ([(qi + 1, 1)] if qi < 31 else []) + [(32, None), (33, None)]
        else:
            kts = [(32, None), (33, None)]
        for ki, (kt, mk) in enumerate(kts):
            b_ = cnt % 2
            cnt += 1
            op("tensor", lambda e, b_=b_, kt=kt, qi=qi: e.matmul(pss[b_][:, :].rearrange("p (h q) -> p h q", h=4), lhsT=kT[:, kt * 128:(kt + 1) * 128],
                                                                rhs=qT[:, :, qi * 128:(qi + 1) * 128], start=True, stop=True),
               reads=["kT", "qT"], writes=["pss%d" % b_])
            op("scalar", lambda e, b_=b_: e.activation(out=E_[b_][:], in_=pss[b_][:], func=AF.Exp, scale=scale),
               reads=["pss%d" % b_], writes=["E%d" % b_])
            if mk is not None:
                def mkf(e, b_=b_, mk=mk):
                    ins = None
                    for h in range(4):
                        ins = e.tensor_tensor(out=E_[b_][:, h * 128:(h + 1) * 128], in0=E_[b_][:, h * 128:(h + 1) * 128], in1=msk[:, mk, :], op=ALU.mult)
                    return ins
                op("vector", mkf, reads=["E%d" % b_, "msk"], writes=["E%d" % b_])

            def pv(e, b_=b_, kt=kt, ki=ki, last=(ki == len(kts) - 1)):
                ins = None
                for h in range(4):
                    ins = e.matmul(po[h // 2][:, (h % 2) * 129:(h % 2) * 129 + 129], lhsT=E_[b_][:, h * 128:(h + 1) * 128], rhs=v1[:, kt, :],
                                   start=(ki == 0 and h % 2 == 0), stop=last)
                return ins
            op("tensor", pv, reads=["E%d" % b_, "v1", "v1o"], writes=["po"])
        for h in range(4):
            pa = po[h // 2][:, (h % 2) * 129:(h % 2) * 129 + 129]
            yb_ = tcnt % 2
            tcnt += 1
            op("vector", lambda e, h=h, pa=pa: e.tensor_tensor(out=den[:, h:h + 1], in0=pa[:, 128:129], in1=esk[:, h:h + 1], op=ALU.add),
               reads=["po", "esk"], writes=["den%d" % h])
            op("vector", lambda e, h=h: e.reciprocal(out=den[:, 4 + h:5 + h], in_=den[:, h:h + 1]), reads=["den%d" % h], writes=["rec%d" % h])
            op("vector", lambda e, h=h, pa=pa, yb_=yb_: e.tensor_scalar(out=ybq[yb_][:], in0=pa[:, 0:128], scalar1=den[:, 4 + h:5 + h], scalar2=None, op0=ALU.mult),
               reads=["po", "rec%d" % h], writes=["ybq%d" % yb_])
            op("tensor", lambda e, yb_=yb_: e.transpose(out=pt[:, yb_ * 128:(yb_ + 1) * 128], in_=ybq[yb_][:], identity=identb[:]),
               reads=["ybq%d" % yb_, "identb"], writes=["pt%d" % yb_])
            op("scalar", lambda e, yb_=yb_, h=h, qi=qi: e.activation(out=ybT[:, h, qi * 128:(qi + 1) * 128], in_=pt[:, yb_ * 128:(yb_ + 1) * 128], func=AF.Copy),
               reads=["pt%d" % yb_], writes=["ybT"])
    for h in range(4):
        op("sync", lambda e, h=h: e.dma_start(out=Y_T[512 + h * 128:512 + (h + 1) * 128, :], in_=ybT[:, h, :]), reads=["ybT"], writes=["yb%d" % h], dma="ybo%d" % h)
    kb.end()

    kb.begin()
    op = kb.op
    li = lambda_init(layer)
    lqk = kb.sb("lqk", [128, 4, 64]); lt = kb.sb("lt", [128, 2, 64]); ls = kb.sb("ls", [128, 4])
    gn = kb.sb("gn", [128, 128])
    identb = kb.sb("identb", [128, 128], BF16)
    qh = kb.sb("qh", [128, TB], BF16); kh = kb.sb("kh", [128, TB], BF16)
    v1h = kb.sb("v1h", [128, 34, 129], BF16)
    ycT = kb.sb("ycT", [128, TB], BF16)
    E_ = [kb.sb("E%d" % i, [128, 512], BF16) for i in range(3)]
    sm = kb.sb("sm", [128, 8])
    y0 = kb.sb("y0", [128, 128]); yy = kb.sb("yy", [128, 128]); sq = kb.sb("sq", [128, 128])
    ynb = [kb.sb("ynb%d" % i, [128, 128], BF16) for i in range(2)]
    pss = [kb.ps("pss%d" % i, [128, 512]) for i in range(2)]
    pacc = [kb.ps("pacc%d" % i, [128, 512]) for i in range(3)]
    pt = kb.ps("pt", [128, 1024], BF16)
    op("sync", lambda e: e.dma_start(out=lqk[:], in_=prm["lqk"]), writes=["lqk"], dma="lqk")
    op("sync", lambda e: e.dma_start(out=gn[:], in_=prm["gn"]), writes=["gn"], dma="gn")
    op("sync", lambda e: e.dma_start(out=identb[:], in_=prm["identb"]), writes=["identb"], dma="idb")
    op("vector", lambda e: e.tensor_scalar(out=gn[:], in0=gn[:], scalar1=float(1.0 - li), scalar2=None, op0=ALU.mult), reads=["gn"], writes=["gn"])
    for k in range(2):
        op("vector", lambda e, k=k: e.tensor_tensor(out=lt[:, k, :], in0=lqk[:, 2 * k, :], in1=lqk[:, 2 * k + 1, :], op=ALU.mult), reads=["lqk"], writes=["lt"])
        op("vector", lambda e, k=k: e.reduce_sum(out=ls[:, k:k + 1], in_=lt[:, k, :], axis=mybir.AxisListType.X), reads=["lt"], writes=["ls"])
    op("scalar", lambda e: e.activation(out=ls[:, 0:2], in_=ls[:, 0:2], func=AF.Exp), reads=["ls"], writes=["ls"])
    op("vector", lambda e: e.tensor_tensor(out=ls[:, 2:3], in0=ls[:, 1:2], in1=ls[:, 0:1], op=ALU.subtract), reads=["ls"], writes=["ls2"])
    op("vector", lambda e: e.tensor_scalar(out=ls[:, 3:4], in0=ls[:, 2:3], scalar1=float(-li), scalar2=None, op0=ALU.add), reads=["ls2"], writes=["neglam"])
    scale = 64 ** -0.5
    cnt = 0
    tcnt = 0
    for h in range(4):
        op("sync", lambda e, h=h: e.dma_start(out=qh[:], in_=FMX[1024 + h * 128:1024 + (h + 1) * 128, :]), writes=["qh"], dma="qh")
        op("sync", lambda e, h=h: e.dma_start(out=kh[:], in_=FMX[1536 + h * 128:1536 + (h + 1) * 128, :]), writes=["kh"], dma="kh")
        op("sync", lambda e, h=h: e.dma_start(out=v1h[:, :, 0:128], in_=VX[:, 128 + h * 128:256 + h * 128].rearrange("(kt p) c -> p kt c", p=128)),
           writes=["v1h"], dma="v1h")
        op("vector", lambda e: e.memset(v1h[:, :, 128:129], 1.0), writes=["v1ho"])
        chunks = [(k * 512, 512, list(range(34))) for k in range(8)] + [(LAT, 256, [32, 33])]
        for (c0, n, ktl) in chunks:
            nsub = n // 128
            for m in range(2):
                for ki, kt in enumerate(ktl):
                    b_ = cnt % 2
                    eb = cnt % 3
                    cnt += 1
                    op("tensor", lambda e, b_=b_, kt=kt, c0=c0, n=n, m=m: e.matmul(
                        pss[b_][:, :n], lhsT=kh[m * 64:(m + 1) * 64, kt * 128:(kt + 1) * 128], rhs=qh[m * 64:(m + 1) * 64, c0:c0 + n], start=True, stop=True),
                       reads=["kh", "qh"], writes=["pss%d" % b_])
                    op("scalar", lambda e, b_=b_, eb=eb, n=n: e.activation(out=E_[eb][:, :n], in_=pss[b_][:, :n], func=AF.Exp, scale=scale),
                       reads=["pss%d" % b_], writes=["E%d" % eb])

                    def pv(e, eb=eb, kt=kt, m=m, nsub=nsub, first=(ki == 0), last=(ki == len(ktl) - 1)):
                        ins = None
                        banks = set()
                        for sub in range(nsub):
                            idx = m * 4 + sub
                            st_ = first and (idx // 3) not in banks
                            banks.add(idx // 3)
                            ins = e.matmul(pacc[idx // 3][:, (idx % 3) * 129:(idx % 3) * 129 + 129], lhsT=E_[eb][:, sub * 128:(sub + 1) * 128],
                                           rhs=v1h[:, kt, :], start=st_, stop=last)
                        return ins
                    op("tensor", pv, reads=["E%d" % eb, "v1h", "v1ho"], writes=["pacc"])
            for sub in range(nsub):
                a0 = pacc[sub // 3][:, (sub % 3) * 129:(sub % 3) * 129 + 129]
                i1 = 4 + sub
                a1 = pacc[i1 // 3][:, (i1 % 3) * 129:(i1 % 3) * 129 + 129]
                yb_ = tcnt % 2
                tcnt += 1
                op("vector", lambda e, a0=a0: e.reciprocal(out=sm[:, 0:1], in_=a0[:, 128:129]), reads=["pacc"], writes=["sm0"])
                op("vector", lambda e, a1=a1: e.reciprocal(out=sm[:, 1:2], in_=a1[:, 128:129]), reads=["pacc"], writes=["sm1"])
                op("vector", lambda e: e.tensor_tensor(out=sm[:, 2:3], in0=sm[:, 1:2], in1=ls[:, 3:4], op=ALU.mult), reads=["sm1", "neglam"], writes=["sm2"])
                op("vector", lambda e, a0=a0: e.tensor_scalar(out=y0[:], in0=a0[:, 0:128], scalar1=sm[:, 0:1], scalar2=None, op0=ALU.mult),
                   reads=["pacc", "sm0"], writes=["y0"])
                op("vector", lambda e, a1=a1: e.scalar_tensor_tensor(out=yy[:], in0=a1[:, 0:128], scalar=sm[:, 2:3], in1=y0[:], op0=ALU.mult, op1=ALU.add),
                   reads=["pacc", "sm2", "y0"], writes=["yy"])
                op("vector", lambda e: e.tensor_tensor(out=sq[:], in0=yy[:], in1=yy[:], op=ALU.mult), reads=["yy"], writes=["sq"])
                op("vector", lambda e: e.reduce_sum(out=sm[:, 3:4], in_=sq[:], axis=mybir.AxisListType.X), reads=["sq"], writes=["sm3"])
                op("vector", lambda e: e.tensor_scalar(out=sm[:, 4:5], in0=sm[:, 3:4], scalar1=1.0 / 128, scalar2=LN_EPS, op0=ALU.mult, op1=ALU.add),
                   reads=["sm3"], writes=["sm4"])
                op("scalar", lambda e: e.activation(out=sm[:, 5:6], in_=sm[:, 4:5], func=AF.Sqrt), reads=["sm4"], writes=["sm5"])
                op("vector", lambda e: e.reciprocal(out=sm[:, 6:7], in_=sm[:, 5:6]), reads=["sm5"], writes=["sm6"])
                op("vector", lambda e: e.tensor_scalar(out=yy[:], in0=yy[:], scalar1=sm[:, 6:7], scalar2=None, op0=ALU.mult), reads=["yy", "sm6"], writes=["yy"])
                op("vector", lambda e, yb_=yb_: e.tensor_tensor(out=ynb[yb_][:], in0=yy[:], in1=gn[:], op=ALU.mult), reads=["yy", "gn"], writes=["ynb%d" % yb_])
                op("tensor", lambda e, yb_=yb_: e.transpose(out=pt[:, yb_ * 128:(yb_ + 1) * 128], in_=ynb[yb_][:], identity=identb[:]),
                   reads=["ynb%d" % yb_, "identb"], writes=["pt%d" % yb_])
                op("scalar", lambda e, yb_=yb_, c0=c0, sub=sub: e.activation(out=ycT[:, c0 + sub * 128:c0 + (sub + 1) * 128], in_=pt[:, yb_ * 128:(yb_ + 1) * 128], func=AF.Copy),
                   reads=["pt%d" % yb_], writes=["ycT"])
        op("sync", lambda e, h=h: e.dma_start(out=Y_T[1024 + h * 128:1024 + (h + 1) * 128, :], in_=ycT[:]), reads=["ycT"], writes=["yc%d" % h], dma="yco")
    kb.end()


def build_p2(layer):
    kb = KB()
    kb.end()
    FMX = kb.din("FMX", [2176, TB], BF16)
    VX = kb.din("VX", [TB, 640], BF16)
    prm = dict(conv_w=kb.din("conv_w", [128, 16]), conv_b=kb.din("conv_b", [128, 4]), lru_p=kb.din("lru_p", [128, 24]),
               lru_w=kb.din("lru_w", [16, 128, 128]), masks=kb.din("masks", [2, 128, 128], BF16), identb=kb.din("identb", [128, 128], BF16),
               sink=kb.din("sink", [128, 4]), lqk=kb.din("lqk", [128, 4, 64]), gn=kb.din("gn", [128, 128]))
    Y_T = kb.dout("Y_T", [1536, TB], BF16)
    p2_body(kb, layer, FMX, VX, Y_T, prm)
    return kb.nc


AX_X = mybir.AxisListType.X


def ln_stats(op, src, n, stats, mv, skey, pfx):
    def st(e):
        ins = None
        for c in range(8):
            ins = e.bn_stats(out=stats[:n, c * 6:(c + 1) * 6], in_=src[:n, c * 512:(c + 1) * 512])
        return ins
    op("vector", st, reads=[skey], writes=[pfx + "stats"])
    op("vector", lambda e: e.bn_aggr(out=mv[:n, 0:2], in_=stats[:n, :]), reads=[pfx + "stats"], writes=[pfx + "mv"])
    op("vector", lambda e: e.tensor_scalar(out=mv[:n, 2:3], in0=mv[:n, 1:2], scalar1=LN_EPS, scalar2=None, op0=ALU.add),
       reads=[pfx + "mv"], writes=[pfx + "rs"])
    op("scalar", lambda e: e.activation(out=mv[:n, 3:4], in_=mv[:n, 2:3], func=AF.Sqrt), reads=[pfx + "rs"], writes=[pfx + "sd"])
    op("vector", lambda e: e.reciprocal(out=mv[:n, 2:3], in_=mv[:n, 3:4]), reads=[pfx + "sd"], writes=[pfx + "rs"])
    return [pfx + "mv", pfx + "rs"]


def p34_body(kb, Y3, G_T, x_own, modT_d, W, X2, scr):
    nc = kb.nc
    M_T, Z, X1, H2T, COMBT, F, MODROW = scr["M_T"], scr["Z"], scr["X1"], scr["H2T"], scr["COMBT"], scr["F"], scr["MODROW"]
    mrow = MODROW.rearrange("(i r) p -> r i p", r=2)

    def modrow_bc(chunk, r):
        return mrow[r, chunk * 32:(chunk + 1) * 32, :].partition_broadcast(128)

    def row_bc(ap_row):
        return ap_row[0, :].partition_broadcast(128)

    kb.begin()
    op = kb.op
    modT = kb.sb("modT", [128, 384]); identf = kb.sb("identf", [128, 128]); mr = kb.sb("mr", [128, 384])
    pm = kb.ps("pm", [128, 512])
    op("sync", lambda e: e.dma_start(out=modT[:], in_=modT_d), writes=["modT"], dma="a0")
    op("sync", lambda e: e.dma_start(out=identf[:], in_=W["identf"]), writes=["identf"], dma="a1")
    for k in range(3):
        op("tensor", lambda e, k=k: e.transpose(out=pm[:, k * 128:(k + 1) * 128], in_=modT[:, k * 128:(k + 1) * 128], identity=identf[:]),
           reads=["modT", "identf"], writes=["pm"])
    op("vector", lambda e: e.tensor_copy(out=mr[:], in_=pm[:, 0:384]), reads=["pm"], writes=["mr"])
    for k in range(3):
        op("sync", lambda e, k=k: e.dma_start(out=MODROW[k * 128:(k + 1) * 128, :], in_=mr[:, k * 128:(k + 1) * 128]), reads=["mr"], writes=["MODROW"], dma="a2")
    kb.end()

    kb.begin()
    op = kb.op
    yT = kb.sb("yT", [128, 48, T], BF16)
    pb = [[kb.sb("pb%d_%d" % (s_, br), [128, 16, 256], BF16) for br in range(3)] for s_ in range(2)]
    gsb = kb.sb("gsb", [128, 3, T], BF16); sig = kb.sb("sig", [128, 3, T])
    t0_ = kb.sb("t0", [128, 512]); t1_ = kb.sb("t1", [128, 512])
    mst = [kb.sb("mst%d" % i, [128, T], BF16) for i in range(2)]
    psb = [[kb.ps("psb%d_%d" % (s_, br), [128, 512]) for br in range(3)] for s_ in range(2)]
    for br in range(3):
        op("sync", lambda e, br=br: e.dma_start(out=yT[:, br * 16:(br + 1) * 16, :], in_=Y3[br * 2048:(br + 1) * 2048, :].rearrange("(kt p) t -> p kt t", p=128)),
           writes=["yT"], dma="y%d" % br)
    cnt = 0
    for pc in range(16):
        sl = pc % 2
        for br in range(3):
            op("gpsimd", lambda e, sl=sl, br=br, pc=pc: e.dma_start(
                out=pb[sl][br][:], in_=W["w_br"][br].rearrange("(kt p) n -> p kt n", p=128)[:, :, pc * 256:(pc + 1) * 256]),
               writes=["pb%d_%d" % (sl, br)], dma="pb%d_%d" % (sl, br))
        for ct2 in range(2):
            dt = pc * 2 + ct2
            for br in range(3):
                op("sync", lambda e, br=br, dt=dt: e.dma_start(out=gsb[:, br, :], in_=G_T[br * 4096 + dt * 128:br * 4096 + (dt + 1) * 128, :]),
                   writes=["gsb"], dma="g%d" % br)
            op("scalar", lambda e: e.activation(out=sig[:], in_=gsb[:], func=AF.Sigmoid), reads=["gsb"], writes=["sig"])
            ms = dt % 2
            for (c0, n) in CHUNKS:
                ps_ = cnt % 2
                cnt += 1
                for br in range(3):
                    def mm(e, sl=sl, br=br, ct2=ct2, c0=c0, n=n, ps_=ps_):
                        ins = None
                        for kt in range(16):
                            ins = e.matmul(psb[ps_][br][:, :n], lhsT=pb[sl][br][:, kt, ct2 * 128:(ct2 + 1) * 128], rhs=yT[:, br * 16 + kt, c0:c0 + n],
                                           start=(kt == 0), stop=(kt == 15))
                        return ins
                    op("tensor", mm, reads=["pb%d_%d" % (sl, br), "yT"], writes=["psb%d_%d" % (ps_, br)])
                rk = ["psb%d_%d" % (ps_, br) for br in range(3)]
                op("vector", lambda e, ps_=ps_, c0=c0, n=n: e.tensor_tensor(out=t0_[:, :n], in0=psb[ps_][0][:, :n], in1=sig[:, 0, c0:c0 + n], op=ALU.mult),
                   reads=[rk[0], "sig"], writes=["t0"])
                op("vector", lambda e, ps_=ps_, c0=c0, n=n: e.tensor_tensor(out=t1_[:, :n], in0=psb[ps_][1][:, :n], in1=sig[:, 1, c0:c0 + n], op=ALU.mult),
                   reads=[rk[1], "sig"], writes=["t1"])
                op("vector", lambda e, n=n: e.tensor_tensor(out=t0_[:, :n], in0=t0_[:, :n], in1=t1_[:, :n], op=ALU.add), reads=["t0", "t1"], writes=["t0"])
                op("vector", lambda e, ps_=ps_, c0=c0, n=n: e.tensor_tensor(out=t1_[:, :n], in0=psb[ps_][2][:, :n], in1=sig[:, 2, c0:c0 + n], op=ALU.mult),
                   reads=[rk[2], "sig", "t0"], writes=["t1"])
                op("vector", lambda e, n=n, c0=c0, ms=ms: e.tensor_tensor(out=mst[ms][:, c0:c0 + n], in0=t0_[:, :n], in1=t1_[:, :n], op=ALU.add),
                   reads=["t0", "t1"], writes=["mst%d" % ms])
            op("sync", lambda e, dt=dt, ms=ms: e.dma_start(out=M_T[dt * 128:(dt + 1) * 128, :], in_=mst[ms][:]), reads=["mst%d" % ms], writes=["M_T"], dma="mst%d" % ms)
    kb.end()

    kb.begin()
    op = kb.op
    mT = kb.sb("mT", [128, 32, T], BF16)
    pan = [kb.sb("pan%d" % i, [128, 32, 512], BF16) for i in range(2)]
    bout = kb.sb("bout", [128, D]); g1bc = [kb.sb("g1bc%d" % r, [128, D]) for r in range(2)]
    zs = [kb.sb("zs%d" % i, [128, 512]) for i in range(2)]
    psz = [kb.ps("psz%d" % i, [128, 512]) for i in range(3)]
    op("sync", lambda e: e.dma_start(out=mT[:], in_=M_T.rearrange("(kt p) t -> p kt t", p=128)), reads=["M_T"], writes=["mT"], dma="mT")
    op("sync", lambda e: e.dma_start(out=bout[:], in_=row_bc(W["b_out"])), writes=["bout"], dma="b0")
    for r in range(2):
        op("sync", lambda e, r=r: e.dma_start(out=g1bc[r][:].rearrange("p (i q) -> p i q", q=128), in_=modrow_bc(2, r)), reads=["MODROW"], writes=["g1bc%d" % r], dma="b%d" % (r + 1))
    wo_v = W["w_out"].rearrange("(kt p) n -> p kt n", p=128)
    cnt = 0
    for pc in range(8):
        sl = pc % 2
        for q in range(4):
            op("gpsimd", lambda e, sl=sl, q=q, pc=pc: e.dma_start(out=pan[sl][:, q * 8:(q + 1) * 8, :], in_=wo_v[:, q * 8:(q + 1) * 8, pc * 512:(pc + 1) * 512]),
               writes=["pan%d_%d" % (sl, q)], dma="pan%d" % sl)
        for tt, (t0, n) in enumerate(TTILES):
            r = 0 if tt < 8 else 1
            pp = cnt % 3
            zb = cnt % 2
            cnt += 1

            def mm(e, sl=sl, t0=t0, n=n, pp=pp):
                ins = None
                for kt in range(32):
                    ins = e.matmul(psz[pp][:n, :], lhsT=mT[:, kt, t0:t0 + n], rhs=pan[sl][:, kt, :], start=(kt == 0), stop=(kt == 31))
                return ins
            op("tensor", mm, reads=["pan%d_%d" % (sl, q_) for q_ in range(4)] + ["mT"], writes=["psz%d" % pp])
            op("vector", lambda e, pp=pp, zb=zb, n=n, pc=pc: e.tensor_tensor(out=zs[zb][:n, :], in0=psz[pp][:n, :], in1=bout[:n, pc * 512:(pc + 1) * 512], op=ALU.add),
               reads=["psz%d" % pp, "bout"], writes=["zs%d" % zb])
            op("vector", lambda e, zb=zb, n=n, pc=pc, r=r: e.tensor_tensor(out=zs[zb][:n, :], in0=zs[zb][:n, :], in1=g1bc[r][:n, pc * 512:(pc + 1) * 512], op=ALU.mult),
               reads=["zs%d" % zb, "g1bc%d" % r], writes=["zs%d" % zb])
            op("sync", lambda e, zb=zb, n=n, t0=t0, pc=pc: e.dma_start(out=Z[t0:t0 + n, pc * 512:(pc + 1) * 512], in_=zs[zb][:n, :]),
               reads=["zs%d" % zb], writes=["Z"], dma="zs%d" % zb)
    kb.end()

    kb.begin()
    op = kb.op
    A = kb.sb("A", [128, D]); Bt = kb.sb("B", [128, D])
    lng = kb.sb("lng", [128, D]); lnb = kb.sb("lnb", [128, D])
    h2f = kb.sb("h2f", [128, 32, 128]); h2b = kb.sb("h2b", [128, 32, 128], BF16)
    modT = kb.sb("modT", [128, 384]); ops2 = kb.sb("ops2", [128, 64])
    identf = kb.sb("identf", [128, 128])
    rw = kb.sb("rw", [128, 32, 32]); rbb = kb.sb("rbb", [128, 32])
    stats = kb.sb("stats", [128, 48]); mv = kb.sb("mv", [128, 4]); mv2 = kb.sb("mv2", [128, 4])
    lg = kb.sb("lg", [128, 32]); mx8 = kb.sb("mx8", [128, 8]); msk = kb.sb("msk", [128, 32]); ex = kb.sb("ex", [128, 32])
    sm = kb.sb("sm", [128, 4]); cT = kb.sb("cT", [32, 128])
    pst = [kb.ps("pst%d" % i, [128, 512]) for i in range(2)]
    psr = kb.ps("psr", [128, 512]); psc = kb.ps("psc", [128, 512])
    op("sync", lambda e: e.dma_start(out=modT[:], in_=modT_d), writes=["modT"], dma="a0")
    op("sync", lambda e: e.dma_start(out=identf[:], in_=W["identf"]), writes=["identf"], dma="a1")
    op("sync", lambda e: e.dma_start(out=lng[:], in_=row_bc(W["ln1_g"])), writes=["lng"], dma="a2")
    op("sync", lambda e: e.dma_start(out=lnb[:], in_=row_bc(W["ln1_b"])), writes=["lnb"], dma="a3")
    op("sync", lambda e: e.dma_start(out=rw[:], in_=W["router_w"].rearrange("(kt p) n -> p kt n", p=128)), writes=["rw"], dma="a4")
    op("sync", lambda e: e.dma_start(out=rbb[:], in_=row_bc(W["router_b"])), writes=["rbb"], dma="a5")
    op("vector", lambda e: e.tensor_scalar(out=ops2[:], in0=modT[:, 256:320], scalar1=1.0, scalar2=None, op0=ALU.add), reads=["modT"], writes=["ops2"])
    modT3 = modT[:].rearrange("p (k r) -> p k r", r=2)
    ops23 = ops2[:].rearrange("p (k r) -> p k r", r=2)
    H2v = H2T.rearrange("(dt p) t -> p dt t", p=128)
    for tt, (t0, n) in enumerate(TTILES):
        r = 0 if tt < 8 else 1
        op("sync", lambda e, t0=t0, n=n: e.dma_start(out=A[:n, :], in_=Z[t0:t0 + n, :]), reads=["Z"], writes=["A"], dma="A")
        op("sync", lambda e, t0=t0, n=n: e.dma_start(out=Bt[:n, :], in_=x_own[t0:t0 + n, :]), writes=["B"], dma="B")
        op("vector", lambda e, n=n: e.scalar_tensor_tensor(out=A[:n, :], in0=Bt[:n, :], scalar=float(ALPHA), in1=A[:n, :], op0=ALU.mult, op1=ALU.add),
           reads=["A", "B"], writes=["A"])
        ks = ln_stats(op, A, n, stats, mv, "A", "s1")
        op("vector", lambda e, n=n: e.tensor_scalar(out=A[:n, :], in0=A[:n, :], scalar1=mv[:n, 0:1], scalar2=mv[:n, 2:3], op0=ALU.subtract, op1=ALU.mult),
           reads=["A"] + ks, writes=["A"])
        op("vector", lambda e, n=n: e.tensor_tensor(out=A[:n, :], in0=A[:n, :], in1=lng[:n, :], op=ALU.mult), reads=["A", "lng"], writes=["A"])
        op("vector", lambda e, n=n: e.tensor_tensor(out=A[:n, :], in0=A[:n, :], in1=lnb[:n, :], op=ALU.add), reads=["A", "lnb"], writes=["A"])
        op("sync", lambda e, t0=t0, n=n: e.dma_start(out=X1[t0:t0 + n, :], in_=A[:n, :]), reads=["A"], writes=["X1"], dma="x1")
        ks2 = ln_stats(op, A, n, stats, mv2, "A", "s2")
        op("vector", lambda e, n=n: e.tensor_scalar(out=Bt[:n, :], in0=A[:n, :], scalar1=mv2[:n, 0:1], scalar2=mv2[:n, 2:3], op0=ALU.subtract, op1=ALU.mult),
           reads=["A", "B"] + ks2, writes=["B"])
        for g in range(8):
            ps_ = pst[g % 2]

            def tr(e, g=g, n=n, ps_=ps_):
                ins = None
                for j in range(4):
                    dt = g * 4 + j
                    ins = e.transpose(out=ps_[:, j * 128:j * 128 + n], in_=Bt[:n, dt * 128:(dt + 1) * 128], identity=identf[:n, :n])
                return ins
            op("tensor", tr, reads=["B", "identf"], writes=["pst%d" % (g % 2)])

            def ev(e, g=g, n=n, ps_=ps_, r=r):
                ins = None
                for j in range(4):
                    dt = g * 4 + j
                    ins = e.tensor_scalar(out=h2f[:, dt, :n], in0=ps_[:, j * 128:j * 128 + n], scalar1=ops23[:, 128 + dt - 128, r:r + 1],
                                          scalar2=modT3[:, 96 + dt, r:r + 1], op0=ALU.mult, op1=ALU.add)
                return ins
            op("vector", ev, reads=["pst%d" % (g % 2), "ops2", "modT"], writes=["h2f"])
        op("scalar", lambda e, n=n: e.activation(out=h2b[:, :, :n], in_=h2f[:, :, :n], func=AF.Copy), reads=["h2f"], writes=["h2b"])
        op("sync", lambda e, t0=t0, n=n: e.dma_start(out=H2v[:, :, t0:t0 + n], in_=h2b[:, :, :n]), reads=["h2b"], writes=["H2T"], dma="h2")

        def rmm(e, n=n):
            ins = None
            for dt in range(32):
                ins = e.matmul(psr[:n, 0:32], lhsT=h2f[:, dt, :n], rhs=rw[:, dt, :], start=(dt == 0), stop=(dt == 31))
            return ins
        op("tensor", rmm, reads=["h2f", "rw"], writes=["psr"])
        op("vector", lambda e, n=n: e.tensor_tensor(out=lg[:n, :], in0=psr[:n, 0:32], in1=rbb[:n, :], op=ALU.add), reads=["psr", "rbb"], writes=["lg"])
        op("vector", lambda e, n=n: e.max(out=mx8[:n, :], in_=lg[:n, :]), reads=["lg"], writes=["mx8"])
        op("vector", lambda e, n=n: e.tensor_scalar(out=msk[:n, :], in0=lg[:n, :], scalar1=mx8[:n, 3:4], scalar2=None, op0=ALU.is_ge), reads=["lg", "mx8"], writes=["msk"])
        op("vector", lambda e, n=n: e.tensor_scalar(out=sm[:n, 0:1], in0=mx8[:n, 0:1], scalar1=-1.0, scalar2=None, op0=ALU.mult), reads=["mx8"], writes=["sm0"])
        op("scalar", lambda e, n=n: e.activation(out=ex[:n, :], in_=lg[:n, :], func=AF.Exp, bias=sm[:n, 0:1]), reads=["lg", "sm0"], writes=["ex"])
        op("vector", lambda e, n=n: e.tensor_tensor(out=ex[:n, :], in0=ex[:n, :], in1=msk[:n, :], op=ALU.mult), reads=["ex", "msk"], writes=["ex"])
        op("vector", lambda e, n=n: e.reduce_sum(out=sm[:n, 1:2], in_=ex[:n, :], axis=AX_X), reads=["ex"], writes=["sm1"])
        op("vector", lambda e, n=n: e.reciprocal(out=sm[:n, 2:3], in_=sm[:n, 1:2]), reads=["sm1"], writes=["sm2"])
        op("vector", lambda e, n=n: e.tensor_scalar(out=ex[:n, :], in0=ex[:n, :], scalar1=sm[:n, 2:3], scalar2=None, op0=ALU.mult), reads=["ex", "sm2"], writes=["ex"])
        op("tensor", lambda e, n=n: e.transpose(out=psc[0:32, 0:n], in_=ex[:n, :], identity=identf[:n, :n]), reads=["ex", "identf"], writes=["psc"])
        op("vector", lambda e, n=n: e.tensor_copy(out=cT[:, :n], in_=psc[0:32, 0:n]), reads=["psc"], writes=["cT"])
        op("sync", lambda e, t0=t0, n=n: e.dma_start(out=COMBT[:, t0:t0 + n], in_=cT[:, :n]), reads=["cT"], writes=["COMBT"], dma="ct")
    kb.end()

    kb.begin()
    op = kb.op
    h2T = kb.sb("h2T", [128, 32, T], BF16)
    acts = kb.sb("acts", [128, 8, T], BF16)
    gp = [kb.sb("gp%d" % i, [128, 32, 128], BF16) for i in range(2)]
    up = [kb.sb("up%d" % i, [128, 32, 128], BF16) for i in range(2)]
    dp = [kb.sb("dp%d" % i, [128, 8, 512], BF16) for i in range(2)]
    cbc = [kb.sb("cbc%d" % i, [128, T]) for i in range(2)]
    bg = kb.sb("bg", [128, 128]); bu = kb.sb("bu", [128, 128])
    cst = kb.sb("cst", [128, 4])
    g1 = [kb.sb("g1_%d" % i, [128, 512]) for i in range(2)]
    sg = [kb.sb("sg_%d" % i, [128, 512]) for i in range(2)]
    u1 = [kb.sb("u1_%d" % i, [128, 512]) for i in range(2)]
    fp_ = [kb.sb("fp%d" % i, [128, 512]) for i in range(2)]
    combT = kb.sb("combT", [32, T]); bdn = kb.sb("bdn", [32, D])
    psg = [kb.ps("psg%d" % i, [128, 512]) for i in range(2)]
    psu = [kb.ps("psu%d" % i, [128, 512]) for i in range(2)]
    psd = [kb.ps("psd%d" % i, [128, 512]) for i in range(3)]
    op("sync", lambda e: e.dma_start(out=h2T[:], in_=H2T.rearrange("(kt p) t -> p kt t", p=128)), reads=["H2T"], writes=["h2T"], dma="h2T")
    op("sync", lambda e: e.dma_start(out=bg[:], in_=W["b_gateT"]), writes=["bg"], dma="c0")
    op("sync", lambda e: e.dma_start(out=bu[:], in_=W["b_upT"]), writes=["bu"], dma="c1")
    op("sync", lambda e: e.dma_start(out=combT[:], in_=COMBT), reads=["COMBT"], writes=["combT"], dma="c2")
    op("sync", lambda e: e.dma_start(out=bdn[:], in_=W["b_down"]), writes=["bdn"], dma="c3")
    op("vector", lambda e: e.memset(cst[:, 0:1], 7.0), writes=["cst0"])
    op("vector", lambda e: e.memset(cst[:, 1:2], -7.0), writes=["cst1"])
    op("vector", lambda e: e.memset(cst[:, 2:3], 1.0), writes=["cst2"])
    CK = ["cst0", "cst1", "cst2"]
    fcnt = 0
    for pc in range(8):
        for tt, (t0, n) in enumerate(TTILES):
            pp = fcnt % 3
            fb = fcnt % 2
            fcnt += 1
            op("tensor", lambda e, pp=pp, t0=t0, n=n, pc=pc: e.matmul(psd[pp][:n, :], lhsT=combT[:, t0:t0 + n], rhs=bdn[:, pc * 512:(pc + 1) * 512], start=True, stop=True),
               reads=["combT", "bdn"], writes=["psd%d" % pp])
            op("vector", lambda e, pp=pp, fb=fb, n=n: e.tensor_copy(out=fp_[fb][:n, :], in_=psd[pp][:n, :]), reads=["psd%d" % pp], writes=["fp%d" % fb])
            op("sync", lambda e, fb=fb, t0=t0, n=n, pc=pc: e.dma_start(out=F[t0:t0 + n, pc * 512:(pc + 1) * 512], in_=fp_[fb][:n, :]),
               reads=["fp%d" % fb], writes=["F%d_%d" % (tt, pc)], dma="fo%d" % fb)
    wg_v = W["w_gate"].rearrange("e (kt p) f -> e p kt f", p=128)
    wu_v = W["w_up"].rearrange("e (kt p) f -> e p kt f", p=128)
    wd_v = W["w_down"].rearrange("(g j) (ft p) n -> g p (j ft) n", j=2, p=128)
    pcnt = 0
    ccnt = 0
    for ex_ in range(32):
        cb_ = ex_ % 2
        op("sync", lambda e, ex_=ex_, cb_=cb_: e.dma_start(out=cbc[cb_][:], in_=COMBT[ex_, :].partition_broadcast(128)), reads=["COMBT"], writes=["cbc%d" % cb_], dma="cbc%d" % cb_)
        for ft in range(4):
            sl = pcnt % 2
            pcnt += 1
            for hq in range(2):
                op("gpsimd", lambda e, sl=sl, ex_=ex_, ft=ft, hq=hq: e.dma_start(out=gp[sl][:, hq * 16:(hq + 1) * 16, :], in_=wg_v[ex_, :, hq * 16:(hq + 1) * 16, ft * 128:(ft + 1) * 128]),
                   writes=["gp%d_%d" % (sl, hq)], dma="gp%d" % sl)
                op("gpsimd", lambda e, sl=sl, ex_=ex_, ft=ft, hq=hq: e.dma_start(out=up[sl][:, hq * 16:(hq + 1) * 16, :], in_=wu_v[ex_, :, hq * 16:(hq + 1) * 16, ft * 128:(ft + 1) * 128]),
                   writes=["up%d_%d" % (sl, hq)], dma="up%d" % sl)
            bi = ex_ * 4 + ft
            for (c0, n) in CHUNKS:
                b_ = ccnt % 2
                ccnt += 1

                def mmg(e, sl=sl, c0=c0, n=n, b_=b_):
                    ins = None
                    for kt in range(32):
                        ins = e.matmul(psg[b_][:, :n], lhsT=gp[sl][:, kt, :], rhs=h2T[:, kt, c0:c0 + n], start=(kt == 0), stop=(kt == 31))
                    return ins

                def mmu(e, sl=sl, c0=c0, n=n, b_=b_):
                    ins = None
                    for kt in range(32):
                        ins = e.matmul(psu[b_][:, :n], lhsT=up[sl][:, kt, :], rhs=h2T[:, kt, c0:c0 + n], start=(kt == 0), stop=(kt == 31))
                    return ins
                op("tensor", mmg, reads=["gp%d_0" % sl, "gp%d_1" % sl, "h2T"], writes=["psg%d" % b_])
                op("tensor", mmu, reads=["up%d_0" % sl, "up%d_1" % sl, "h2T"], writes=["psu%d" % b_])
                op("vector", lambda e, b_=b_, n=n, bi=bi: e.tensor_scalar(out=g1[b_][:, :n], in0=psg[b_][:, :n], scalar1=bg[:, bi:bi + 1], scalar2=cst[:, 0:1], op0=ALU.add, op1=ALU.min),
                   reads=["psg%d" % b_, "bg"] + CK, writes=["g1_%d" % b_])
                op("scalar", lambda e, b_=b_, n=n: e.activation(out=sg[b_][:, :n], in_=g1[b_][:, :n], func=AF.Sigmoid, scale=1.702), reads=["g1_%d" % b_], writes=["sg_%d" % b_])
                op("vector", lambda e, b_=b_, n=n, bi=bi: e.tensor_scalar(out=u1[b_][:, :n], in0=psu[b_][:, :n], scalar1=bu[:, bi:bi + 1], scalar2=cst[:, 0:1], op0=ALU.add, op1=ALU.min),
                   reads=["psu%d" % b_, "bu"] + CK, writes=["u1_%d" % b_])
                op("vector", lambda e, b_=b_, n=n: e.tensor_scalar(out=u1[b_][:, :n], in0=u1[b_][:, :n], scalar1=cst[:, 1:2], scalar2=cst[:, 2:3], op0=ALU.max, op1=ALU.add),
                   reads=["u1_%d" % b_] + CK, writes=["u1_%d" % b_])
                op("vector", lambda e, b_=b_, n=n: e.tensor_tensor(out=g1[b_][:, :n], in0=g1[b_][:, :n], in1=sg[b_][:, :n], op=ALU.mult), reads=["g1_%d" % b_, "sg_%d" % b_], writes=["g1_%d" % b_])
                op("vector", lambda e, b_=b_, n=n: e.tensor_tensor(out=g1[b_][:, :n], in0=g1[b_][:, :n], in1=u1[b_][:, :n], op=ALU.mult), reads=["g1_%d" % b_, "u1_%d" % b_], writes=["g1_%d" % b_])
                j = (ex_ % 2) * 4 + ft
                op("vector", lambda e, b_=b_, n=n, c0=c0, j=j, cb_=cb_: e.tensor_tensor(out=acts[:, j, c0:c0 + n], in0=g1[b_][:, :n], in1=cbc[cb_][:, c0:c0 + n], op=ALU.mult),
                   reads=["g1_%d" % b_, "cbc%d" % cb_], writes=["acts"])
        if ex_ % 2 == 1:
            grp = ex_ // 2
            for pc in range(8):
                ds_ = (grp * 8 + pc) % 2
                op("gpsimd", lambda e, ds_=ds_, grp=grp, pc=pc: e.dma_start(out=dp[ds_][:], in_=wd_v[grp, :, :, pc * 512:(pc + 1) * 512]), writes=["dp%d" % ds_], dma="dp%d" % ds_)
                for tt, (t0, n) in enumerate(TTILES):
                    pp = fcnt % 3
                    fb = fcnt % 2
                    fcnt += 1
                    fk = "F%d_%d" % (tt, pc)

                    def mmd(e, ds_=ds_, t0=t0, n=n, pp=pp):
                        ins = None
                        for j in range(8):
                            ins = e.matmul(psd[pp][:n, :], lhsT=acts[:, j, t0:t0 + n], rhs=dp[ds_][:, j, :], start=(j == 0), stop=(j == 7))
                        return ins
                    op("tensor", mmd, reads=["acts", "dp%d" % ds_], writes=["psd%d" % pp])
                    op("sync", lambda e, fb=fb, t0=t0, n=n, pc=pc: e.dma_start(out=fp_[fb][:n, :], in_=F[t0:t0 + n, pc * 512:(pc + 1) * 512]),
                       reads=[fk], writes=["fp%d" % fb], dma="fi%d" % fb)
                    op("vector", lambda e, pp=pp, fb=fb, n=n: e.tensor_tensor(out=fp_[fb][:n, :], in0=psd[pp][:n, :], in1=fp_[fb][:n, :], op=ALU.add),
                       reads=["psd%d" % pp, "fp%d" % fb], writes=["fp%d" % fb])
                    op("sync", lambda e, fb=fb, t0=t0, n=n, pc=pc: e.dma_start(out=F[t0:t0 + n, pc * 512:(pc + 1) * 512], in_=fp_[fb][:n, :]),
                       reads=["fp%d" % fb], writes=[fk], dma="fo%d" % fb)
    kb.end()

    kb.begin()
    op = kb.op
    A = kb.sb("A", [128, D]); Bt = kb.sb("B", [128, D])
    lng = kb.sb("lng", [128, D]); lnb = kb.sb("lnb", [128, D])
    g2bc = [kb.sb("g2bc%d" % r, [128, D]) for r in range(2)]
    stats = kb.sb("stats", [128, 48]); mv = kb.sb("mv", [128, 4])
    op("sync", lambda e: e.dma_start(out=lng[:], in_=row_bc(W["ln2_g"])), writes=["lng"], dma="a2")
    op("sync", lambda e: e.dma_start(out=lnb[:], in_=row_bc(W["ln2_b"])), writes=["lnb"], dma="a3")
    for r in range(2):
        op("sync", lambda e, r=r: e.dma_start(out=g2bc[r][:].rearrange("p (i q) -> p i q", q=128), in_=modrow_bc(5, r)), writes=["g2bc%d" % r], dma="b%d" % r)
    for tt, (t0, n) in enumerate(TTILES):
        r = 0 if tt < 8 else 1
        op("sync", lambda e, t0=t0, n=n: e.dma_start(out=A[:n, :], in_=F[t0:t0 + n, :]), writes=["A"], dma="A")
        op("sync", lambda e, t0=t0, n=n: e.dma_start(out=Bt[:n, :], in_=X1[t0:t0 + n, :]), writes=["B"], dma="B")
        op("vector", lambda e, n=n, r=r: e.tensor_tensor(out=A[:n, :], in0=A[:n, :], in1=g2bc[r][:n, :], op=ALU.mult), reads=["A", "g2bc%d" % r], writes=["A"])
        op("vector", lambda e, n=n: e.scalar_tensor_tensor(out=A[:n, :], in0=Bt[:n, :], scalar=float(ALPHA), in1=A[:n, :], op0=ALU.mult, op1=ALU.add),
           reads=["A", "B"], writes=["A"])
        ks = ln_stats(op, A, n, stats, mv, "A", "s1")
        op("vector", lambda e, n=n: e.tensor_scalar(out=A[:n, :], in0=A[:n, :], scalar1=mv[:n, 0:1], scalar2=mv[:n, 2:3], op0=ALU.subtract, op1=ALU.mult),
           reads=["A"] + ks, writes=["A"])
        op("vector", lambda e, n=n: e.tensor_tensor(out=A[:n, :], in0=A[:n, :], in1=lng[:n, :], op=ALU.mult), reads=["A", "lng"], writes=["A"])
        op("vector", lambda e, n=n: e.tensor_tensor(out=A[:n, :], in0=A[:n, :], in1=lnb[:n, :], op=ALU.add), reads=["A", "lnb"], writes=["A"])
        op("sync", lambda e, t0=t0, n=n: e.dma_start(out=X2[t0:t0 + n, :], in_=A[:n, :]), reads=["A"], writes=["X2"], dma="x2")
    kb.end()


def build_p34(nblk=2, dbg=False):
    kb = KB()
    kb.end()
    nc = kb.nc
    Y3 = kb.din("Y3", [nblk * 6144, T], BF16)
    G_T = kb.din("G_T", [nblk * 12288, T], BF16)
    x_own = kb.din("x", [nblk * T, D])
    modT_d = kb.din("modT", [128, 384])
    W = dict(identf=kb.din("identf", [128, 128]), w_br=kb.din("w_br", [3, 2048, D]), w_out=kb.din("w_out", [D, D]), b_out=kb.din("b_out", [1, D]),
             ln1_g=kb.din("ln1_g", [1, D]), ln1_b=kb.din("ln1_b", [1, D]), router_w=kb.din("router_w", [D, 32]), router_b=kb.din("router_b", [1, 32]),
             w_gate=kb.din("w_gate", [32, D, 512]), w_up=kb.din("w_up", [32, D, 512]), w_down=kb.din("w_down", [32, 512, D]),
             b_gateT=kb.din("b_gateT", [128, 128]), b_upT=kb.din("b_upT", [128, 128]), b_down=kb.din("b_down", [32, D]),
             ln2_g=kb.din("ln2_g", [1, D]), ln2_b=kb.din("ln2_b", [1, D]))
    X2 = kb.dout("X2", [nblk * T, D])
    kind = "ExternalOutput" if dbg else "Internal"
    def scr_t(name, shape, dt):
        return nc.dram_tensor(name, list(shape), dt, kind=kind).ap()
    scr = dict(M_T=scr_t("M_T", [D, T], BF16), Z=scr_t("Z", [T, D], F32), X1=scr_t("X1", [T, D], F32), H2T=scr_t("H2T", [D, T], BF16),
               COMBT=scr_t("COMBT", [32, T], F32), F=scr_t("F", [T, D], F32), MODROW=scr_t("MODROW", [384, 128], F32))
    for blk in range(nblk):
        p34_body(kb, Y3[blk * 6144:(blk + 1) * 6144, :], G_T[blk * 12288:(blk + 1) * 12288, :], x_own[blk * T:(blk + 1) * T, :], modT_d, W,
                 X2[blk * T:(blk + 1) * T, :], scr)
    return kb.nc


def fmT(v, r=None):
    return np.ascontiguousarray(v.reshape(-1, 128).T)

def pm_inmaps(inp):
    rows = np.stack([inp['c'][0], inp['c'][1], inp['c_ctx']], 0)
    cT = np.ascontiguousarray(rows.reshape(3, 32, 128).transpose(2, 1, 0)).reshape(128, 96)
    maps = []
    for i in range(8):
        w = np.ascontiguousarray(inp['w_ada'][:, :, i*3072:(i+1)*3072]).reshape(2 * D, 3072)
        bb = inp['b_ada'][:, i*3072:(i+1)*3072].reshape(2, 24, 128)
        b_adaT = np.ascontiguousarray(bb.transpose(2, 0, 1)).reshape(128, 48)
        maps.append(dict(cT=cT, w_ada=w, b_adaT=b_adaT))
    return maps

def pm_assemble(res):
    full = np.concatenate([np.asarray(r['modS']).reshape(128, 2, 24, 3) for r in res], 2)
    out = {}
    for l in range(2):
        for b in range(2):
            out[(l, b)] = np.ascontiguousarray(np.stack([full[:, l, :, b], full[:, l, :, 2]], -1)).reshape(128, 384)
    return out

def p1_inmaps(inp, l, x_own, modTs):
    maps = []
    ident = np.eye(128, dtype=np.float32)
    psw = swap_perms()
    for i in range(4):
        blks = (2 * i, 2 * i + 1)
        b = blks[0] // 4
        maps.append(dict(x=np.concatenate([x_own[k] for k in blks], 0), modT=modTs[(l, b)], w_in=inp['w_in'][l],
                         rope=np.concatenate([rope_tables(k % 4) for k in blks], 0), psw=psw, ident=ident))
    return maps

def p1_split(res, modTs, l):
    out = []
    for k in range(8):
        r = res[k // 2]
        o = k % 2
        out.append(dict(FM=np.asarray(r['FM'])[o*20992:(o+1)*20992], VA=np.asarray(r['VA'])[o*T:(o+1)*T], VD=np.asarray(r['VD'])[o*T:(o+1)*T],
                        modT=modTs[(l, k // 4)]))
    return out

def own_tokens(inp):
    xs = []
    for i in range(8):
        b, j = divmod(i, 4)
        xs.append(np.concatenate([inp['x'][b, j*1024:(j+1)*1024], inp['ctx'][b, j*64:(j+1)*64]], 0))
    return xs


def p2_params(inp, l, g):
    cw = inp['conv_w'][l][:, g*512:(g+1)*512]
    conv_w = np.ascontiguousarray(cw.reshape(4, 4, 128).transpose(2, 1, 0)).reshape(128, 16)
    conv_b = fmT(inp['conv_b'][l][g*512:(g+1)*512])
    def dirvec(a):
        s = a[:, g*512:(g+1)*512].reshape(2, 4, 128)
        return np.ascontiguousarray(s.transpose(2, 0, 1)).reshape(128, 8)
    lru_p = np.concatenate([dirvec(inp['lru_br'][l]), dirvec(inp['lru_bi'][l]), dirvec(inp['lru_lambda'][l])], 1)
    wr = inp['lru_wr'][l][:, 4*g:4*g+4].reshape(8, 128, 128)
    wi = inp['lru_wi'][l][:, 4*g:4*g+4].reshape(8, 128, 128)
    lru_w = np.ascontiguousarray(np.concatenate([wr, wi], 0))
    k = np.arange(128)[:, None]; q = np.arange(128)[None, :]
    masks = np.stack([(k >= q), (k <= q)], 0).astype(np.float32).astype(BF)
    sink = np.ascontiguousarray(np.broadcast_to(inp['wa_sink'][l][4*g:4*g+4][None], (128, 4)))
    lqk = np.stack([inp['da_lq1'][l], inp['da_lk1'][l], inp['da_lq2'][l], inp['da_lk2'][l]], 0)
    lqk = np.ascontiguousarray(np.broadcast_to(lqk[None], (128, 4, 64)))
    gn = np.ascontiguousarray(np.broadcast_to(inp['da_norm_g'][l][None], (128, 128)))
    return dict(conv_w=conv_w, conv_b=conv_b, lru_p=np.ascontiguousarray(lru_p), lru_w=lru_w, masks=masks,
                identb=np.eye(128, dtype=np.float32).astype(BF), sink=sink, lqk=lqk, gn=gn)

def p2_inmaps(inp, l, p1res):
    maps = []
    for i in range(8):
        b, g = divmod(i, 4)
        srcs = [p1res[4*b + j] for j in range(4)]
        def rows(r0, n):
            lat = np.concatenate([s['FM'][r0:r0+n, 0:1024] for s in srcs], 1)
            cx = np.concatenate([s['FM'][r0:r0+n, 1024:1088] for s in srcs], 1)
            return np.concatenate([lat, cx], 1)
        FMX = np.concatenate([rows(g*512, 512), rows(2048 + g*512, 512), rows(4608 + g*512, 512), rows(6656 + g*512, 512), rows(4096 + g*128, 128)], 0)
        def vrows(key, c0, n):
            lat = np.concatenate([s[key][0:1024, c0:c0+n] for s in srcs], 0)
            cx = np.concatenate([s[key][1024:1088, c0:c0+n] for s in srcs], 0)
            return np.concatenate([lat, cx], 0)
        VX = np.concatenate([vrows('VA', g*128, 128), vrows('VD', g*512, 512)], 1)
        m = dict(FMX=np.ascontiguousarray(FMX), VX=np.ascontiguousarray(VX))
        m.update(p2_params(inp, l, g))
        maps.append(m)
    return maps


def p34_weights(inp, l):
    bgT = np.ascontiguousarray(inp['exp_b_gate'][l].reshape(32, 4, 128).transpose(2, 0, 1)).reshape(128, 128)
    buT = np.ascontiguousarray(inp['exp_b_up'][l].reshape(32, 4, 128).transpose(2, 0, 1)).reshape(128, 128)
    return dict(identf=np.eye(128, dtype=np.float32), w_br=np.stack([inp['w_br_a'][l], inp['w_br_b'][l], inp['w_br_c'][l]], 0),
                w_out=inp['w_out'][l], b_out=inp['b_out'][l][None], ln1_g=inp['ln1_g'][l][None], ln1_b=inp['ln1_b'][l][None],
                router_w=inp['router_w'][l], router_b=inp['router_b'][l][None], w_gate=inp['exp_w_gate'][l], w_up=inp['exp_w_up'][l],
                w_down=inp['exp_w_down'][l], b_gateT=bgT, b_upT=buT, b_down=inp['exp_b_down'][l], ln2_g=inp['ln2_g'][l][None], ln2_b=inp['ln2_b'][l][None])

def p34_inmaps(inp, l, p1res, p2res, x_own, nblk=1):
    wts = p34_weights(inp, l)
    maps = []
    for i in range(8 // nblk):
        Y3s, Gs, xs = [], [], []
        for k in range(i * nblk, (i + 1) * nblk):
            b, j = divmod(k, 4)
            parts = []
            for br in range(3):
                for g in range(4):
                    Y = p2res[4*b + g]['Y_T']
                    parts.append(np.concatenate([Y[br*512:(br+1)*512, j*1024:(j+1)*1024], Y[br*512:(br+1)*512, 4096 + j*64:4096 + (j+1)*64]], 1))
            Y3s.append(np.concatenate(parts, 0)); Gs.append(p1res[k]['FM'][8704:]); xs.append(x_own[k])
        m = dict(Y3=np.ascontiguousarray(np.concatenate(Y3s, 0)), G_T=np.ascontiguousarray(np.concatenate(Gs, 0)),
                 x=np.ascontiguousarray(np.concatenate(xs, 0)), modT=p1res[i * nblk]['modT'])
        m.update(wts)
        maps.append(m)
    return maps

def p34_split(res, nblk=1):
    return [np.asarray(res[k // nblk]['X2'])[(k % nblk) * T:(k % nblk + 1) * T] for k in range(8)]


_PROGS = {}


def _prog(key, fn):
    if key not in _PROGS:
        _PROGS[key] = fn()
    return _PROGS[key]


def _run(nc, maps):
    res = _bu.run_bass_kernel_spmd(nc, maps, core_ids=list(range(len(maps))))
    return res.results


def kernel(**inp):
    inp = {k: np.asarray(v) for k, v in inp.items()}
    x_own = own_tokens(inp)
    modTs = pm_assemble(_run(_prog("pm", build_pm), pm_inmaps(inp)))
    for l in range(2):
        p1res = p1_split(_run(_prog("p1", build_p1), p1_inmaps(inp, l, x_own, modTs)), modTs, l)
        r2 = _run(_prog("p2_%d" % l, lambda l=l: build_p2(l)), p2_inmaps(inp, l, p1res))
        p2res = [dict(Y_T=np.asarray(r["Y_T"])) for r in r2]
        del r2
        x_own = p34_split(_run(_prog("p34", lambda: build_p34(1)), p34_inmaps(inp, l, p1res, p2res, x_own, 1)), 1)
        del p1res, p2res
    out = np.empty((2, 4096, 4096), np.float32)
    for i in range(8):
        b, j = divmod(i, 4)
        out[b, j * 1024:(j + 1) * 1024] = x_own[i][0:1024]
    return out
```

```python
import contextlib
import numpy as np
import ml_dtypes
import concourse.bass as bass
import concourse.mybir as mybir
import concourse.bass_utils as _bu

F32, BF16 = mybir.dt.float32, mybir.dt.bfloat16
AF = mybir.ActivationFunctionType
ALU = mybir.AluOpType
BF = ml_dtypes.bfloat16

D = 4096
T = 1088
NL = 1024
NCX = 64
TB = 4352
D_IN = 23552
ALPHA = 4 ** 0.25
LN_EPS = 1e-5
CHUNKS = [(0, 512), (512, 512), (1024, 64)]
TTILES = [(i * 128, 128) for i in range(8)] + [(1024, 64)]

ENGS = ("tensor", "vector", "scalar", "gpsimd", "sync")


class Sched:
    def __init__(self, nc):
        self.nc = nc
        self.ops = []
        self.last_w = {}
        self.readers = {}

    def op(self, eng, fn, reads=(), writes=(), dma=None):
        i = len(self.ops)
        deps = set()
        for r in reads:
            w = self.last_w.get(r)
            if w is not None:
                deps.add(w)
        for w_ in writes:
            w = self.last_w.get(w_)
            if w is not None:
                deps.add(w)
            for rd in self.readers.get(w_, ()):
                deps.add(rd)
        deps.discard(i)
        if eng == "tensor":
            deps = {d for d in deps if self.ops[d]["eng"] != "tensor"}
        self.ops.append(dict(eng=eng, fn=fn, deps=deps, dma=dma, sig=False))
        for r in reads:
            self.readers.setdefault(r, []).append(i)
        for w_ in writes:
            self.last_w[w_] = i
            self.readers[w_] = []
        return i

    PH = 0

    def emit(self):
        Sched.PH += 1
        nc = self.nc
        ops = self.ops
        for o in ops:
            for d in o["deps"]:
                ops[d]["sig"] = True
        for o in ops:
            if o["dma"] is not None:
                o["sig"] = True
        for eng in ENGS:
            my = [o for o in ops if o["eng"] == eng and o["dma"] is None]
            if my:
                my[-1]["sig"] = True
        counts = {}
        epoch_ctr = {}
        for o in ops:
            if not o["sig"]:
                continue
            if o["dma"] is not None:
                k = ("dma", o["dma"])
                inc = 16
            else:
                ep = epoch_ctr.get(o["eng"], 0) // 20000
                epoch_ctr[o["eng"]] = epoch_ctr.get(o["eng"], 0) + 1
                k = ("eng", o["eng"], ep)
                inc = 1
            counts[k] = counts.get(k, 0) + inc
            o["semk"] = k
            o["semv"] = counts[k]
            o["inc"] = inc
        with contextlib.ExitStack() as es:
            sems = {}
            for k in counts:
                sems[k] = es.enter_context(nc.semaphore("s%d_" % Sched.PH + "_".join(str(x) for x in k)))
            block = es.enter_context(nc.Block())
            for eng in ENGS:
                my = [o for o in ops if o["eng"] == eng]

                def body(e, my=my, eng=eng):
                    seen = {}
                    for o in my:
                        need = {}
                        for d in o["deps"]:
                            od = ops[d]
                            k, v = od["semk"], od["semv"]
                            if need.get(k, 0) < v:
                                need[k] = v
                        for k, v in need.items():
                            if seen.get(k, 0) >= v:
                                continue
                            e.wait_ge(sems[k], v)
                            seen[k] = v
                        ins = o["fn"](e)
                        if o["sig"]:
                            ins.then_inc(sems[o["semk"]], o["inc"])
                    for k, v in counts.items():
                        if seen.get(k, 0) < v:
                            e.wait_ge(sems[k], v)

                getattr(block, eng)(body)


class KB:
    def __init__(self):
        KB.NID = 0
        Sched.PH = 0
        self.nc = bass.Bass("TRN2", target_bir_lowering=False)
        self.begin()

    def begin(self):
        self.s = Sched(self.nc)
        self.es = contextlib.ExitStack()

    def end(self):
        self.s.emit()
        self.es.close()

    def din(self, name, shape, dt=F32):
        return self.nc.dram_tensor(name, list(shape), dt, kind="ExternalInput").ap()

    def dout(self, name, shape, dt=F32):
        return self.nc.dram_tensor(name, list(shape), dt, kind="ExternalOutput").ap()

    NID = 0

    def sb(self, name, shape, dt=F32):
        KB.NID += 1
        return self.es.enter_context(self.nc.sbuf_tensor("%s_u%d" % (name, KB.NID), list(shape), dt))

    def ps(self, name, shape, dt=F32):
        KB.NID += 1
        return self.es.enter_context(self.nc.psum_tensor("%s_u%d" % (name, KB.NID), list(shape), dt))

    def op(self, *a, **k):
        return self.s.op(*a, **k)

    def finish(self):
        self.end()
        return self.nc


def rev(ap, n):
    return bass.AP(ap.tensor, ap.offset + n - 1, [ap.ap[0], [-1, n]])


def build_pm():
    kb = KB()
    nc = kb.nc
    cT = kb.din("cT", [128, 96])
    wada = kb.din("w_ada", [2 * D, 3072])
    bada = kb.din("b_adaT", [128, 48])
    mod_o = kb.dout("modS", [128, 144])
    pan = [kb.sb("pan%d" % i, [128, 32, 512], BF16) for i in range(2)]
    cTs = kb.sb("cTs", [128, 96]); sT = kb.sb("sT", [128, 96], BF16)
    bsb = kb.sb("bsb", [128, 48]); msb = kb.sb("msb", [128, 144])
    psm = kb.ps("psm", [128, 512])
    op = kb.op
    op("sync", lambda e: e.dma_start(out=cTs[:], in_=cT), writes=["cTs"], dma="c0")
    op("sync", lambda e: e.dma_start(out=bsb[:], in_=bada), writes=["bsb"], dma="c1")
    op("scalar", lambda e: e.activation(out=sT[:], in_=cTs[:], func=AF.Silu), reads=["cTs"], writes=["sT"])
    sT3 = sT[:].rearrange("p (k r) -> p k r", r=3)
    wv = wada.rearrange("(l kt p) n -> l p kt n", l=2, p=128)
    for pi in range(12):
        l, pj = divmod(pi, 6)
        sl = pi % 2
        for q in range(4):
            op("gpsimd", lambda e, sl=sl, q=q, l=l, pj=pj: e.dma_start(out=pan[sl][:, q * 8:(q + 1) * 8, :], in_=wv[l, :, q * 8:(q + 1) * 8, pj * 512:(pj + 1) * 512]),
               writes=["pan%d_%d" % (sl, q)], dma="pan%d" % sl)

        def mm(e, sl=sl, pi=pi):
            ins = None
            for ct in range(4):
                idx = pi * 4 + ct
                for kt in range(32):
                    ins = e.matmul(psm[:, idx * 3:idx * 3 + 3], lhsT=pan[sl][:, kt, ct * 128:(ct + 1) * 128], rhs=sT3[:, kt, :], start=(kt == 0), stop=(kt == 31))
            return ins
        op("tensor", mm, reads=["pan%d_%d" % (sl, q_) for q_ in range(4)] + ["sT"], writes=["psm"])
    m3 = msb[:].rearrange("p (k r) -> p k r", r=3)
    p3 = psm[:, 0:144].rearrange("p (k r) -> p k r", r=3)
    for r in range(3):
        op("vector", lambda e, r=r: e.tensor_tensor(out=m3[:, :, r], in0=p3[:, :, r], in1=bsb[:], op=ALU.add), reads=["psm", "bsb"], writes=["msb"])
    op("sync", lambda e: e.dma_start(out=mod_o, in_=msb[:]), reads=["msb"], writes=["mod_o"], dma="mo")
    return kb.finish()


def build_p1(nblk=2):
    kb = KB()
    kb.end()
    nc = kb.nc
    x_a = kb.din("x", [nblk * T, D])
    modT_d = kb.din("modT", [128, 384])
    win = kb.din("w_in", [D, D_IN])
    tabs_a = kb.din("rope", [nblk * 4, 128, T])
    psw_d = kb.din("psw", [2, 128, 128], BF16)
    ident_d = kb.din("ident", [128, 128])
    FM_a = kb.dout("FM", [nblk * 20992, T], BF16)
    VA_a = kb.dout("VA", [nblk * T, 512], BF16)
    VD_a = kb.dout("VD", [nblk * T, 2048], BF16)
    for blk in range(nblk):
        p1_body(kb, x_a[blk * T:(blk + 1) * T, :], modT_d, win, tabs_a[blk * 4:(blk + 1) * 4], psw_d, ident_d,
                FM_a[blk * 20992:(blk + 1) * 20992, :], VA_a[blk * T:(blk + 1) * T, :], VD_a[blk * T:(blk + 1) * T, :])
    return kb.nc


def p1_body(kb, x, modT_d, win, tabs_d, psw_d, ident_d, FM_o, VA_o, VD_o):
    kb.begin()
    hT = kb.sb("hT", [128, 32, T], BF16)
    pan = [kb.sb("pan%d" % i, [128, 32, 512], BF16) for i in range(2)]
    tabs = kb.sb("tabs", [128, 4, T])
    psw = kb.sb("psw_sb", [128, 2, 128], BF16)
    ident = kb.sb("ident_sb", [128, 128])
    modT = kb.sb("modT_sb", [128, 384])
    ops1 = kb.sb("ops1", [128, 64])
    xt = kb.sb("xt", [128, D])
    xn = kb.sb("xn", [128, D])
    stats = kb.sb("stats", [128, 48])
    mv = kb.sb("mv", [128, 4])
    stg = [kb.sb("stg%d" % i, [128, T], BF16) for i in range(2)]
    stv = [kb.sb("stv%d" % i, [128, 512], BF16) for i in range(2)]
    qb = [kb.sb("qb%d" % i, [128, 512], BF16) for i in range(2)]
    t1 = [kb.sb("t1_%d" % i, [128, 512]) for i in range(2)]
    t2 = [kb.sb("t2_%d" % i, [128, 512]) for i in range(2)]

    pst = [kb.ps("pst%d" % i, [128, 512]) for i in range(2)]
    psp = [kb.ps("psp%d" % i, [128, 512]) for i in range(3)]
    psr = [kb.ps("psr%d" % i, [128, 512]) for i in range(2)]

    op = kb.op
    op("sync", lambda e: e.dma_start(out=modT[:], in_=modT_d), writes=["modT"], dma="c0")
    op("sync", lambda e: e.dma_start(out=ident[:], in_=ident_d), writes=["ident"], dma="c2")
    op("sync", lambda e: e.dma_start(out=tabs[:], in_=tabs_d.rearrange("a p t -> p a t")), writes=["tabs"], dma="c3")
    op("sync", lambda e: e.dma_start(out=psw[:], in_=psw_d.rearrange("a p t -> p a t")), writes=["psw"], dma="c4")
    modT3 = modT[:].rearrange("p (k r) -> p k r", r=2)
    op("vector", lambda e: e.tensor_scalar(out=ops1[:], in0=modT[:, 64:128], scalar1=1.0, scalar2=None, op0=ALU.add),
       reads=["modT"], writes=["ops1"])
    ops13 = ops1[:].rearrange("p (k r) -> p k r", r=2)

    for tt, (t0, n) in enumerate(TTILES):
        r = 0 if tt < 8 else 1
        op("sync", lambda e, t0=t0, n=n: e.dma_start(out=xt[:n, :], in_=x[t0:t0 + n, :]), writes=["xt"], dma="xt")

        def st(e, n=n):
            ins = None
            for c in range(8):
                ins = e.bn_stats(out=stats[:n, c * 6:(c + 1) * 6], in_=xt[:n, c * 512:(c + 1) * 512])
            return ins
        op("vector", st, reads=["xt"], writes=["stats"])
        op("vector", lambda e, n=n: e.bn_aggr(out=mv[:n, 0:2], in_=stats[:n, :]), reads=["stats"], writes=["mv"])
        op("vector", lambda e, n=n: e.tensor_scalar(out=mv[:n, 2:3], in0=mv[:n, 1:2], scalar1=LN_EPS, scalar2=None,
                                                    op0=ALU.add), reads=["mv"], writes=["rs"])
        op("scalar", lambda e, n=n: e.activation(out=mv[:n, 3:4], in_=mv[:n, 2:3], func=AF.Sqrt), reads=["rs"], writes=["sd"])
        op("vector", lambda e, n=n: e.reciprocal(out=mv[:n, 2:3], in_=mv[:n, 3:4]), reads=["sd"], writes=["rs"])
        op("vector", lambda e, n=n: e.tensor_scalar(out=xn[:n, :], in0=xt[:n, :], scalar1=mv[:n, 0:1], scalar2=mv[:n, 2:3],
                                                    op0=ALU.subtract, op1=ALU.mult), reads=["xt", "mv", "rs"], writes=["xn"])
        for g in range(8):
            ps_ = pst[g % 2]

            def tr(e, g=g, n=n, ps_=ps_):
                ins = None
                for j in range(4):
                    dt = g * 4 + j
                    ins = e.transpose(out=ps_[:, j * 128:j * 128 + n], in_=xn[:n, dt * 128:(dt + 1) * 128],
                                      identity=ident[:n, :n])
                return ins
            op("tensor", tr, reads=["xn", "ident"], writes=["pst%d" % (g % 2)])

            def ev(e, g=g, n=n, ps_=ps_, t0=t0, r=r):
                ins = None
                for j in range(4):
                    dt = g * 4 + j
                    ins = e.tensor_scalar(out=hT[:, dt, t0:t0 + n], in0=ps_[:, j * 128:j * 128 + n],
                                          scalar1=ops13[:, dt, r:r + 1], scalar2=modT3[:, dt, r:r + 1],
                                          op0=ALU.mult, op1=ALU.add)
                return ins
            op("vector", ev, reads=["pst%d" % (g % 2), "ops1", "modT"], writes=["hT"])

    win_v = win.rearrange("(kt p) n -> p kt n", p=128)
    def fm_row(col):
        if col < 4608:
            return col
        if col < 5120:
            return None
        if col < 9216:
            return col - 512
        if col < 11264:
            return None
        return col - 2560
    cnt = 0
    for pi in range(46):
        sl = pi % 2
        for q in range(4):
            op("gpsimd", lambda e, sl=sl, q=q, pi=pi: e.dma_start(
                out=pan[sl][:, q * 8:(q + 1) * 8, :], in_=win_v[:, q * 8:(q + 1) * 8, pi * 512:(pi + 1) * 512]),
               writes=["pan%d_%d" % (sl, q)], dma="pan%d" % sl)
        col0 = pi * 512
        if fm_row(col0) is None:
            for tt, (t0, n) in enumerate(TTILES):
                pp = psp[cnt % 3]
                pk = "psp%d" % (cnt % 3)

                def mmv(e, sl=sl, t0=t0, n=n, pp=pp):
                    ins = None
                    for kt in range(32):
                        ins = e.matmul(pp[:n, :], lhsT=hT[:, kt, t0:t0 + n], rhs=pan[sl][:, kt, :],
                                       start=(kt == 0), stop=(kt == 31))
                    return ins
                op("tensor", mmv, reads=["pan%d_%d" % (sl, q_) for q_ in range(4)] + ["hT"], writes=[pk])
                sv = cnt % 2
                op("scalar", lambda e, pp=pp, n=n, sv=sv: e.activation(out=stv[sv][:n, :], in_=pp[:n, :], func=AF.Copy),
                   reads=[pk], writes=["stv%d" % sv])
                if col0 < 5120:
                    dst = VA_o[t0:t0 + n, :]
                else:
                    c = col0 - 9216
                    dst = VD_o[t0:t0 + n, c:c + 512]
                op("sync", lambda e, dst=dst, sv=sv, n=n: e.dma_start(out=dst, in_=stv[sv][:n, :]),
                   reads=["stv%d" % sv], writes=["vout%d" % cnt], dma="stv%d" % sv)
                cnt += 1
            continue
        rope = None
        if 2048 <= col0 < 4608:
            rope = 0
        elif 5120 <= col0 < 9216:
            rope = 1
        for ct in range(4):
            row0 = fm_row(col0) + ct * 128
            sg = (pi * 4 + ct) % 2
            for (c0, n) in CHUNKS:
                pp = psp[cnt % 3]
                pk = "psp%d" % (cnt % 3)

                def mmf(e, sl=sl, ct=ct, c0=c0, n=n, pp=pp):
                    ins = None
                    for kt in range(32):
                        ins = e.matmul(pp[:, :n], lhsT=pan[sl][:, kt, ct * 128:(ct + 1) * 128], rhs=hT[:, kt, c0:c0 + n],
                                       start=(kt == 0), stop=(kt == 31))
                    return ins
                op("tensor", mmf, reads=["pan%d_%d" % (sl, q_) for q_ in range(4)] + ["hT"], writes=[pk])
                if rope is None:
                    if cnt % 2 == 0:
                        op("scalar", lambda e, pp=pp, n=n, sg=sg, c0=c0: e.activation(out=stg[sg][:, c0:c0 + n], in_=pp[:, :n], func=AF.Copy),
                           reads=[pk], writes=["stg%d" % sg])
                    else:
                        op("vector", lambda e, pp=pp, n=n, sg=sg, c0=c0: e.tensor_copy(out=stg[sg][:, c0:c0 + n], in_=pp[:, :n]),
                           reads=[pk], writes=["stg%d" % sg])
                else:
                    rb = cnt % 2
                    op("scalar", lambda e, pp=pp, n=n, rb=rb: e.activation(out=qb[rb][:, :n], in_=pp[:, :n], func=AF.Copy),
                       reads=[pk], writes=["qb%d" % rb])
                    op("tensor", lambda e, rb=rb, n=n, rope=rope: e.matmul(psr[rb][:, :n], lhsT=psw[:, rope, :], rhs=qb[rb][:, :n],
                                                                         start=True, stop=True),
                       reads=["qb%d" % rb, "psw"], writes=["psr%d" % rb])
                    op("vector", lambda e, rb=rb, n=n, c0=c0, rope=rope: e.tensor_tensor(
                        out=t1[rb][:, :n], in0=qb[rb][:, :n], in1=tabs[:, 2 * rope, c0:c0 + n], op=ALU.mult),
                       reads=["qb%d" % rb, "tabs"], writes=["t1_%d" % rb])
                    op("vector", lambda e, rb=rb, n=n, c0=c0, rope=rope: e.tensor_tensor(
                        out=t2[rb][:, :n], in0=psr[rb][:, :n], in1=tabs[:, 2 * rope + 1, c0:c0 + n], op=ALU.mult),
                       reads=["psr%d" % rb, "tabs"], writes=["t2_%d" % rb])
                    op("vector", lambda e, rb=rb, n=n, c0=c0, sg=sg: e.tensor_tensor(
                        out=stg[sg][:, c0:c0 + n], in0=t1[rb][:, :n], in1=t2[rb][:, :n], op=ALU.add),
                       reads=["t1_%d" % rb, "t2_%d" % rb], writes=["stg%d" % sg])
                cnt += 1
            op("sync", lambda e, row0=row0, sg=sg: e.dma_start(out=FM_o[row0:row0 + 128, :], in_=stg[sg][:, :]),
               reads=["stg%d" % sg], writes=["fm%d" % row0], dma="stg%d" % sg)
    kb.end()


def rope_tables(j):
    pos = np.arange(j * NL, (j + 1) * NL)
    prow = (pos // 64).astype(np.float32)
    pcol = (pos % 64).astype(np.float32)
    out = np.zeros((4, 128, T), np.float32)
    out[0, :, NL:] = 1.0
    out[2, :, NL:] = 1.0
    for ti, hd in ((0, 128), (1, 64)):
        n = hd // 4
        inv = (10000.0 ** (-np.arange(n, dtype=np.float32) / n)).astype(np.float32)
        ang = np.concatenate([prow[:, None] * inv, pcol[:, None] * inv], -1)
        cos, sin = np.cos(ang).T, np.sin(ang).T
        half = hd // 2
        for p in range(128):
            dd = p % hd
            if dd < half:
                out[2 * ti, p, :NL] = cos[dd]
                out[2 * ti + 1, p, :NL] = -sin[dd]
            else:
                out[2 * ti, p, :NL] = cos[dd - half]
                out[2 * ti + 1, p, :NL] = sin[dd - half]
    return out


def swap_perms():
    out = np.zeros((2, 128, 128), np.float32)
    for m in range(128):
        out[0, (m + 64) % 128, m] = 1.0
        k = m + 32 if (m % 64) < 32 else m - 32
        out[1, k, m] = 1.0
    return out.astype(BF)


import math


def lambda_init(layer):
    return 0.8 - 0.6 * math.exp(-0.3 * layer)


def p2_body(kb, layer, FMX, VX, Y_T, prm):
    nc = kb.nc
    op = kb.op
    LAT = 4096
    kb.begin()
    op = kb.op
    cw = kb.sb("cw", [128, 16]); cb = kb.sb("cb", [128, 4])
    lp = kb.sb("lp", [128, 24])
    cl = kb.sb("cl", [128, 8]); cl2 = kb.sb("cl2", [128, 8]); tmp8 = kb.sb("tmp8", [128, 8])
    ones = kb.sb("ones", [128, 1])
    wri = kb.sb("wri", [128, 16, 128], BF16)
    upad = kb.sb("upad", [128, LAT + 3], BF16); cpad = kb.sb("cpad", [128, 256 + 3], BF16)
    v32 = kb.sb("v32", [128, TB]); vb = kb.sb("vb", [128, TB], BF16)
    hf = kb.sb("hf", [128, TB]); hr = kb.sb("hr", [128, TB])
    yst = kb.sb("yst", [128, TB], BF16)
    tr_ = [kb.sb("tr%d" % i, [128, 512]) for i in range(2)]
    ti_ = [kb.sb("ti%d" % i, [128, 512]) for i in range(2)]
    ta_ = [kb.sb("ta%d" % i, [128, 512]) for i in range(2)]
    tg_ = [kb.sb("tg%d" % i, [128, 512]) for i in range(2)]
    tb_ = [kb.sb("tb%d" % i, [128, 512]) for i in range(2)]
    psr = [kb.ps("psr%d" % i, [128, 512]) for i in range(2)]
    psi = [kb.ps("psi%d" % i, [128, 512]) for i in range(2)]
    op("sync", lambda e: e.dma_start(out=cw[:], in_=prm["conv_w"]), writes=["cw"], dma="k0")
    op("sync", lambda e: e.dma_start(out=cb[:], in_=prm["conv_b"]), writes=["cb"], dma="k1")
    op("sync", lambda e: e.dma_start(out=lp[:], in_=prm["lru_p"]), writes=["lp"], dma="k2")
    op("gpsimd", lambda e: e.dma_start(out=wri[:], in_=prm["lru_w"].rearrange("a p e -> p a e")), writes=["wri"], dma="k3")
    op("vector", lambda e: e.memset(ones[:], 1.0), writes=["ones"])
    op("scalar", lambda e: e.activation(out=tmp8[:], in_=lp[:, 16:24], func=AF.Exp, scale=-1.0), reads=["lp"], writes=["tmp8"])
    op("scalar", lambda e: e.activation(out=cl[:], in_=tmp8[:], func=AF.Ln, bias=ones[:, 0:1]), reads=["tmp8", "ones"], writes=["cl0"])
    op("vector", lambda e: e.tensor_scalar(out=cl2[:], in0=cl[:], scalar1=-16.0, scalar2=None, op0=ALU.mult), reads=["cl0"], writes=["cl2"])
    op("vector", lambda e: e.tensor_scalar(out=cl[:], in0=cl[:], scalar1=-8.0, scalar2=None, op0=ALU.mult), reads=["cl0", "cl2"], writes=["cl"])
    for ct in range(4):
        op("vector", lambda e: e.memset(upad[:], 0.0), writes=["upad"])
        op("vector", lambda e: e.memset(cpad[:], 0.0), writes=["cpad"])
        op("sync", lambda e, ct=ct: e.dma_start(out=upad[:, 1:1 + LAT], in_=FMX[ct * 128:(ct + 1) * 128, 0:LAT]), writes=["upad"], dma="up")
        op("sync", lambda e, ct=ct: e.dma_start(out=cpad[:, 1:257], in_=FMX[ct * 128:(ct + 1) * 128, LAT:TB]), writes=["cpad"], dma="cp")
        for (src, L, o0, key) in ((upad, LAT, 0, "upad"), (cpad, 256, LAT, "cpad")):
            op("vector", lambda e, src=src, L=L, o0=o0, ct=ct: e.tensor_scalar(
                out=v32[:, o0:o0 + L], in0=src[:, 0:L], scalar1=cw[:, ct * 4:ct * 4 + 1], scalar2=cb[:, ct:ct + 1],
                op0=ALU.mult, op1=ALU.add), reads=[key, "cw", "cb"], writes=["v32"])
            for k in range(1, 4):
                op("vector", lambda e, src=src, L=L, o0=o0, ct=ct, k=k: e.scalar_tensor_tensor(
                    out=v32[:, o0:o0 + L], in0=src[:, k:k + L], scalar=cw[:, ct * 4 + k:ct * 4 + k + 1], in1=v32[:, o0:o0 + L],
                    op0=ALU.mult, op1=ALU.add), reads=[key, "cw", "v32"], writes=["v32"])
        op("scalar", lambda e: e.activation(out=vb[:], in_=v32[:], func=AF.Copy), reads=["v32"], writes=["vb"])
        cc = 0
        for d in range(2):
            H = hf if d == 0 else hr
            hk = "hf" if d == 0 else "hr"
            if d == 0:
                order = [(LAT, 256, None)] + [(k * 512, 512, None) for k in range(8)]
            else:
                order = [(LAT, 256, None)] + [(k * 512, 512, None) for k in range(7, -1, -1)]
            prev = None
            for (c0, n, _) in order:
                b_ = cc % 2
                cc += 1
                j = d * 4 + ct
                op("tensor", lambda e, b_=b_, c0=c0, n=n, j=j: e.matmul(psr[b_][:, :n], lhsT=wri[:, j, :], rhs=vb[:, c0:c0 + n], start=True, stop=True),
                   reads=["wri", "vb"], writes=["psr%d" % b_])
                op("tensor", lambda e, b_=b_, c0=c0, n=n, j=j: e.matmul(psi[b_][:, :n], lhsT=wri[:, 8 + j, :], rhs=vb[:, c0:c0 + n], start=True, stop=True),
                   reads=["wri", "vb"], writes=["psi%d" % b_])
                op("scalar", lambda e, b_=b_, n=n, j=j: e.activation(out=tr_[b_][:, :n], in_=psr[b_][:, :n], func=AF.Sigmoid, bias=lp[:, j:j + 1]),
                   reads=["psr%d" % b_, "lp"], writes=["tr%d" % b_])
                op("scalar", lambda e, b_=b_, n=n, j=j: e.activation(out=ti_[b_][:, :n], in_=psi[b_][:, :n], func=AF.Sigmoid, bias=lp[:, 8 + j:9 + j]),
                   reads=["psi%d" % b_, "lp"], writes=["ti%d" % b_])
                op("scalar", lambda e, b_=b_, n=n, j=j: e.activation(out=ta_[b_][:, :n], in_=tr_[b_][:, :n], func=AF.Exp, scale=cl[:, j:j + 1]),
                   reads=["tr%d" % b_, "cl"], writes=["ta%d" % b_])
                op("scalar", lambda e, b_=b_, n=n, j=j: e.activation(out=tg_[b_][:, :n], in_=tr_[b_][:, :n], func=AF.Exp, scale=cl2[:, j:j + 1]),
                   reads=["tr%d" % b_, "cl2"], writes=["tg%d" % b_])
                op("scalar", lambda e, b_=b_, n=n: e.activation(out=tg_[b_][:, :n], in_=tg_[b_][:, :n], func=AF.Sqrt, bias=ones[:, 0:1], scale=-1.0),
                   reads=["tg%d" % b_, "ones"], writes=["tg%d" % b_])
                op("vector", lambda e, b_=b_, n=n, c0=c0: e.tensor_tensor(out=tb_[b_][:, :n], in0=ti_[b_][:, :n], in1=v32[:, c0:c0 + n], op=ALU.mult),
                   reads=["ti%d" % b_, "v32"], writes=["tb%d" % b_])
                op("vector", lambda e, b_=b_, n=n: e.tensor_tensor(out=tb_[b_][:, :n], in0=tb_[b_][:, :n], in1=tg_[b_][:, :n], op=ALU.mult),
                   reads=["tb%d" % b_, "tg%d" % b_], writes=["tb%d" % b_])
                if d == 0:
                    init = 0.0 if prev is None else H[:, prev:prev + 1]
                    op("vector", lambda e, b_=b_, n=n, c0=c0, init=init, H=H: e.tensor_tensor_scan(
                        out=H[:, c0:c0 + n], data0=ta_[b_][:, :n], data1=tb_[b_][:, :n], initial=init, op0=ALU.mult, op1=ALU.add),
                       reads=["ta%d" % b_, "tb%d" % b_, hk], writes=[hk])
                    prev = c0 + n - 1
                else:
                    init = 0.0 if prev is None else H[:, prev:prev + 1]
                    op("vector", lambda e, b_=b_, n=n, c0=c0, init=init, H=H: e.tensor_tensor_scan(
                        out=rev(H[:, c0:c0 + n], n), data0=rev(ta_[b_][:, :n], n), data1=rev(tb_[b_][:, :n], n), initial=init,
                        op0=ALU.mult, op1=ALU.add), reads=["ta%d" % b_, "tb%d" % b_, hk], writes=[hk])
                    prev = c0
        op("vector", lambda e: e.tensor_tensor(out=yst[:], in0=hf[:], in1=hr[:], op=ALU.add), reads=["hf", "hr"], writes=["yst"])
        op("sync", lambda e, ct=ct: e.dma_start(out=Y_T[ct * 128:(ct + 1) * 128, :], in_=yst[:]), reads=["yst"], writes=["ya%d" % ct], dma="yst")
    kb.end()

    kb.begin()
    op = kb.op
    qT = kb.sb("qT", [128, 4, TB], BF16); kT = kb.sb("kT", [128, TB], BF16)
    v1 = kb.sb("v1", [128, 34, 129], BF16)
    ybT = kb.sb("ybT", [128, 4, TB], BF16)
    msk = kb.sb("msk", [128, 2, 128], BF16)
    identb = kb.sb("identb", [128, 128], BF16)
    esk = kb.sb("esk", [128, 4])
    E_ = [kb.sb("E%d" % i, [128, 512], BF16) for i in range(2)]
    den = kb.sb("den", [128, 8])
    ybq = [kb.sb("ybq%d" % i, [128, 128], BF16) for i in range(2)]
    pss = [kb.ps("pss%d" % i, [128, 512]) for i in range(2)]
    po = [kb.ps("po%d" % i, [128, 512]) for i in range(2)]
    pt = kb.ps("pt", [128, 1024], BF16)
    for h in range(4):
        op("sync", lambda e, h=h: e.dma_start(out=qT[:, h, :], in_=FMX[512 + h * 128:512 + (h + 1) * 128, :]), writes=["qT"], dma="q%d" % h)
    op("sync", lambda e: e.dma_start(out=kT[:], in_=FMX[2048:2176, :]), writes=["kT"], dma="kT")
    op("sync", lambda e: e.dma_start(out=v1[:, :, 0:128], in_=VX[:, 0:128].rearrange("(kt p) c -> p kt c", p=128)), writes=["v1"], dma="v1")
    op("vector", lambda e: e.memset(v1[:, :, 128:129], 1.0), writes=["v1o"])
    op("sync", lambda e: e.dma_start(out=msk[:], in_=prm["masks"].rearrange("a p t -> p a t")), writes=["msk"], dma="msk")
    op("sync", lambda e: e.dma_start(out=identb[:], in_=prm["identb"]), writes=["identb"], dma="idb")
    op("sync", lambda e: e.dma_start(out=esk[:], in_=prm["sink"]), writes=["esk"], dma="esk")
    op("scalar", lambda e: e.activation(out=esk[:], in_=esk[:], func=AF.Exp), reads=["esk"], writes=["esk"])
    scale = 128 ** -0.5
    cnt = 0
    tcnt = 0
    for qi in range(34):
        if qi < 32:
            kts = ([(qi - 1, 0)] if qi > 0 else []) + [(qi, None)] + ([(qi + 1, 1)] if qi < 31 else []) + [(32, None), (33, None)]
        else:
            kts = [(32, None), (33, None)]
        for ki, (kt, mk) in enumerate(kts):
            b_ = cnt % 2
            cnt += 1
            op("tensor", lambda e, b_=b_, kt=kt, qi=qi: e.matmul(pss[b_][:, :].rearrange("p (h q) -> p h q", h=4), lhsT=kT[:, kt * 128:(kt + 1) * 128],
                                                                rhs=qT[:, :, qi * 128:(qi + 1) * 128], start=True, stop=True),
               reads=["kT", "qT"], writes=["pss%d" % b_])
            op("scalar", lambda e, b_=b_: e.activation(out=E_[b_][:], in_=pss[b_][:], func=AF.Exp, scale=scale),
               reads=["pss%d" % b_], writes=["E%d" % b_])
            if mk is not None:
                def mkf(e, b_=b_, mk=mk):
                    ins = None
                    for h in range(4):
                        ins = e.tensor_tensor(out=E_[b_][:, h * 128:(h + 1) * 128], in0=E_[b_][:, h * 128:(h + 1) * 128], in1=msk[:, mk, :], op=ALU.mult)
                    return ins
                op("vector", mkf, reads=["E%d" % b_, "msk"], writes=["E%d" % b_])

            def pv(e, b_=b_, kt=kt, ki=ki, last=(ki == len(kts) - 1)):
                ins = None
                for h in range(4):
                    ins = e.matmul(po[h // 2][:, (h % 2) * 129:(h % 2) * 129 + 129], lhsT=E_[b_][:, h * 128:(h + 1) * 128], rhs=v1[:, kt, :],
                                   start=(ki == 0 and h % 2 == 0), stop=last)
                return ins
            op("tensor", pv, reads=["E%d" % b_, "v1", "v1o"], writes=["po"])
        for h in range(4):
            pa = po[h // 2][:, (h % 2) * 129:(h % 2) * 129 + 129]
            yb_ = tcnt % 2
            tcnt += 1
            op("vector", lambda e, h=h, pa=pa: e.tensor_tensor(out=den[:, h:h + 1], in0=pa[:, 128:129], in1=esk[:, h:h + 1], op=ALU.add),
               reads=["po", "esk"], writes=["den%d" % h])
            op("vector", lambda e, h=h: e.reciprocal(out=den[:, 4 + h:5 + h], in_=den[:, h:h + 1]), reads=["den%d" % h], writes=["rec%d" % h])
            op("vector", lambda e, h=h, pa=pa, yb_=yb_: e.tensor_scalar(out=ybq[yb_][:], in0=pa[:, 0:128], scalar1=den[:, 4 + h:5 + h], scalar2=None, op0=ALU.mult),
               reads=["po", "rec%d" % h], writes=["ybq%d" % yb_])
            op("tensor", lambda e, yb_=yb_: e.transpose(out=pt[:, yb_ * 128:(yb_ + 1) * 128], in_=ybq[yb_][:], identity=identb[:]),
               reads=["ybq%d" % yb_, "identb"], writes=["pt%d" % yb_])
            op("scalar", lambda e, yb_=yb_, h=h, qi=qi: e.activation(out=ybT[:, h, qi * 128:(qi + 1) * 128], in_=pt[:, yb_ * 128:(yb_ + 1) * 128], func=AF.Copy),
               reads=["pt%d" % yb_], writes=["ybT"])
    for h in range(4):
        op("sync", lambda e, h=h: e.dma_start(out=Y_T[512 + h * 128:512 + (h + 1) * 128, :], in_=ybT[:, h, :]), reads=["ybT"], writes=["yb%d" % h], dma="ybo%d" % h)
    kb.end()

    kb.begin()
    op = kb.op
    li = lambda_init(layer)
    lqk = kb.sb("lqk", [128, 4, 64]); lt = kb.sb("lt", [128, 2, 64]); ls = kb.sb("ls", [128, 4])
    gn = kb.sb("gn", [128, 128])
    identb = kb.sb("identb", [128, 128], BF16)
    qh = kb.sb("qh", [128, TB], BF16); kh = kb.sb("kh", [128, TB], BF16)
    v1h = kb.sb("v1h", [128, 34, 129], BF16)
    ycT = kb.sb("ycT", [128, TB], BF16)
    E_ = [kb.sb("E%d" % i, [128, 512], BF16) for i in range(3)]
    sm = kb.sb("sm", [128, 8])
    y0 = kb.sb("y0", [128, 128]); yy = kb.sb("yy", [128, 128]); sq = kb.sb("sq", [128, 128])
    ynb = [kb.sb("ynb%d" % i, [128, 128], BF16) for i in range(2)]
    pss = [kb.ps("pss%d" % i, [128, 512]) for i in range(2)]
    pacc = [kb.ps("pacc%d" % i, [128, 512]) for i in range(3)]
    pt = kb.ps("pt", [128, 1024], BF16)
    op("sync", lambda e: e.dma_start(out=lqk[:], in_=prm["lqk"]), writes=["lqk"], dma="lqk")
    op("sync", lambda e: e.dma_start(out=gn[:], in_=prm["gn"]), writes=["gn"], dma="gn")
    op("sync", lambda e: e.dma_start(out=identb[:], in_=prm["identb"]), writes=["identb"], dma="idb")
    op("vector", lambda e: e.tensor_scalar(out=gn[:], in0=gn[:], scalar1=float(1.0 - li), scalar2=None, op0=ALU.mult), reads=["gn"], writes=["gn"])
    for k in range(2):
        op("vector", lambda e, k=k: e.tensor_tensor(out=lt[:, k, :], in0=lqk[:, 2 * k, :], in1=lqk[:, 2 * k + 1, :], op=ALU.mult), reads=["lqk"], writes=["lt"])
        op("vector", lambda e, k=k: e.reduce_sum(out=ls[:, k:k + 1], in_=lt[:, k, :], axis=mybir.AxisListType.X), reads=["lt"], writes=["ls"])
    op("scalar", lambda e: e.activation(out=ls[:, 0:2], in_=ls[:, 0:2], func=AF.Exp), reads=["ls"], writes=["ls"])
    op("vector", lambda e: e.tensor_tensor(out=ls[:, 2:3], in0=ls[:, 1:2], in1=ls[:, 0:1], op=ALU.subtract), reads=["ls"], writes=["ls2"])
    op("vector", lambda e: e.tensor_scalar(out=ls[:, 3:4], in0=ls[:, 2:3], scalar1=float(-li), scalar2=None, op0=ALU.add), reads=["ls2"], writes=["neglam"])
    scale = 64 ** -0.5
    cnt = 0
    tcnt = 0
    for h in range(4):
        op("sync", lambda e, h=h: e.dma_start(out=qh[:], in_=FMX[1024 + h * 128:1024 + (h + 1) * 128, :]), writes=["qh"], dma="qh")
        op("sync", lambda e, h=h: e.dma_start(out=kh[:], in_=FMX[1536 + h * 128:1536 + (h + 1) * 128, :]), writes=["kh"], dma="kh")
        op("sync", lambda e, h=h: e.dma_start(out=v1h[:, :, 0:128], in_=VX[:, 128 + h * 128:256 + h * 128].rearrange("(kt p) c -> p kt c", p=128)),
           writes=["v1h"], dma="v1h")
        op("vector", lambda e: e.memset(v1h[:, :, 128:129], 1.0), writes=["v1ho"])
        chunks = [(k * 512, 512, list(range(34))) for k in range(8)] + [(LAT, 256, [32, 33])]
        for (c0, n, ktl) in chunks:
            nsub = n // 128
            its = [(m, ki, kt) for m in range(2) for ki, kt in enumerate(ktl)]
            slots = []
            for _ in its:
                slots.append((cnt % 2, cnt % 3))
                cnt += 1

            def emit_S(i):
                m, ki, kt = its[i]
                b_, eb = slots[i]
                op("tensor", lambda e, b_=b_, kt=kt, c0=c0, n=n, m=m: e.matmul(
                    pss[b_][:, :n], lhsT=kh[m * 64:(m + 1) * 64, kt * 128:(kt + 1) * 128], rhs=qh[m * 64:(m + 1) * 64, c0:c0 + n], start=True, stop=True),
                   reads=["kh", "qh"], writes=["pss%d" % b_])

            def emit_E(i):
                b_, eb = slots[i]
                op("scalar", lambda e, b_=b_, eb=eb, n=n: e.activation(out=E_[eb][:, :n], in_=pss[b_][:, :n], func=AF.Exp, scale=scale),
                   reads=["pss%d" % b_], writes=["E%d" % eb])

            def emit_PV(i):
                m, ki, kt = its[i]
                b_, eb = slots[i]

                def pv(e, eb=eb, kt=kt, m=m, nsub=nsub, first=(ki == 0), last=(ki == len(ktl) - 1)):
                    ins = None
                    banks = set()
                    for sub in range(nsub):
                        idx = m * 4 + sub
                        st_ = first and (idx // 3) not in banks
                        banks.add(idx // 3)
                        ins = e.matmul(pacc[idx // 3][:, (idx % 3) * 129:(idx % 3) * 129 + 129], lhsT=E_[eb][:, sub * 128:(sub + 1) * 128],
                                       rhs=v1h[:, kt, :], start=st_, stop=last)
                    return ins
                op("tensor", pv, reads=["E%d" % eb, "v1h", "v1ho"], writes=["pacc"])

            emit_S(0)
            for i in range(len(its)):
                emit_E(i)
                if i + 1 < len(its):
                    emit_S(i + 1)
                emit_PV(i)
            for sub in range(nsub):
                a0 = pacc[sub // 3][:, (sub % 3) * 129:(sub % 3) * 129 + 129]
                i1 = 4 + sub
                a1 = pacc[i1 // 3][:, (i1 % 3) * 129:(i1 % 3) * 129 + 129]
                yb_ = tcnt % 2
                tcnt += 1
                op("vector", lambda e, a0=a0: e.reciprocal(out=sm[:, 0:1], in_=a0[:, 128:129]), reads=["pacc"], writes=["sm0"])
                op("vector", lambda e, a1=a1: e.reciprocal(out=sm[:, 1:2], in_=a1[:, 128:129]), reads=["pacc"], writes=["sm1"])
                op("vector", lambda e: e.tensor_tensor(out=sm[:, 2:3], in0=sm[:, 1:2], in1=ls[:, 3:4], op=ALU.mult), reads=["sm1", "neglam"], writes=["sm2"])
                op("vector", lambda e, a0=a0: e.tensor_scalar(out=y0[:], in0=a0[:, 0:128], scalar1=sm[:, 0:1], scalar2=None, op0=ALU.mult),
                   reads=["pacc", "sm0"], writes=["y0"])
                op("vector", lambda e, a1=a1: e.scalar_tensor_tensor(out=yy[:], in0=a1[:, 0:128], scalar=sm[:, 2:3], in1=y0[:], op0=ALU.mult, op1=ALU.add),
                   reads=["pacc", "sm2", "y0"], writes=["yy"])
                op("vector", lambda e: e.tensor_tensor(out=sq[:], in0=yy[:], in1=yy[:], op=ALU.mult), reads=["yy"], writes=["sq"])
                op("vector", lambda e: e.reduce_sum(out=sm[:, 3:4], in_=sq[:], axis=mybir.AxisListType.X), reads=["sq"], writes=["sm3"])
                op("vector", lambda e: e.tensor_scalar(out=sm[:, 4:5], in0=sm[:, 3:4], scalar1=1.0 / 128, scalar2=LN_EPS, op0=ALU.mult, op1=ALU.add),
                   reads=["sm3"], writes=["sm4"])
                op("scalar", lambda e: e.activation(out=sm[:, 5:6], in_=sm[:, 4:5], func=AF.Sqrt), reads=["sm4"], writes=["sm5"])
                op("vector", lambda e: e.reciprocal(out=sm[:, 6:7], in_=sm[:, 5:6]), reads=["sm5"], writes=["sm6"])
                op("vector", lambda e: e.tensor_scalar(out=yy[:], in0=yy[:], scalar1=sm[:, 6:7], scalar2=None, op0=ALU.mult), reads=["yy", "sm6"], writes=["yy"])
                op("vector", lambda e, yb_=yb_: e.tensor_tensor(out=ynb[yb_][:], in0=yy[:], in1=gn[:], op=ALU.mult), reads=["yy", "gn"], writes=["ynb%d" % yb_])
                op("tensor", lambda e, yb_=yb_: e.transpose(out=pt[:, yb_ * 128:(yb_ + 1) * 128], in_=ynb[yb_][:], identity=identb[:]),
                   reads=["ynb%d" % yb_, "identb"], writes=["pt%d" % yb_])
                op("scalar", lambda e, yb_=yb_, c0=c0, sub=sub: e.activation(out=ycT[:, c0 + sub * 128:c0 + (sub + 1) * 128], in_=pt[:, yb_ * 128:(yb_ + 1) * 128], func=AF.Copy),
                   reads=["pt%d" % yb_], writes=["ycT"])
        op("sync", lambda e, h=h: e.dma_start(out=Y_T[1024 + h * 128:1024 + (h + 1) * 128, :], in_=ycT[:]), reads=["ycT"], writes=["yc%d" % h], dma="yco")
    kb.end()


def build_p2(layer):
    kb = KB()
    kb.end()
    FMX = kb.din("FMX", [2176, TB], BF16)
    VX = kb.din("VX", [TB, 640], BF16)
    prm = dict(conv_w=kb.din("conv_w", [128, 16]), conv_b=kb.din("conv_b", [128, 4]), lru_p=kb.din("lru_p", [128, 24]),
               lru_w=kb.din("lru_w", [16, 128, 128]), masks=kb.din("masks", [2, 128, 128], BF16), identb=kb.din("identb", [128, 128], BF16),
               sink=kb.din("sink", [128, 4]), lqk=kb.din("lqk", [128, 4, 64]), gn=kb.din("gn", [128, 128]))
    Y_T = kb.dout("Y_T", [1536, TB], BF16)
    p2_body(kb, layer, FMX, VX, Y_T, prm)
    return kb.nc


AX_X = mybir.AxisListType.X


def ln_stats(op, src, n, stats, mv, skey, pfx):
    def st(e):
        ins = None
        for c in range(8):
            ins = e.bn_stats(out=stats[:n, c * 6:(c + 1) * 6], in_=src[:n, c * 512:(c + 1) * 512])
        return ins
    op("vector", st, reads=[skey], writes=[pfx + "stats"])
    op("vector", lambda e: e.bn_aggr(out=mv[:n, 0:2], in_=stats[:n, :]), reads=[pfx + "stats"], writes=[pfx + "mv"])
    op("vector", lambda e: e.tensor_scalar(out=mv[:n, 2:3], in0=mv[:n, 1:2], scalar1=LN_EPS, scalar2=None, op0=ALU.add),
       reads=[pfx + "mv"], writes=[pfx + "rs"])
    op("scalar", lambda e: e.activation(out=mv[:n, 3:4], in_=mv[:n, 2:3], func=AF.Sqrt), reads=[pfx + "rs"], writes=[pfx + "sd"])
    op("vector", lambda e: e.reciprocal(out=mv[:n, 2:3], in_=mv[:n, 3:4]), reads=[pfx + "sd"], writes=[pfx + "rs"])
    return [pfx + "mv", pfx + "rs"]


def p34_body(kb, Y3, G_T, x_own, modT_d, W, X2, scr):
    nc = kb.nc
    M_T, Z, X1, H2T, COMBT, F, MODROW = scr["M_T"], scr["Z"], scr["X1"], scr["H2T"], scr["COMBT"], scr["F"], scr["MODROW"]
    mrow = MODROW.rearrange("(i r) p -> r i p", r=2)

    def modrow_bc(chunk, r):
        return mrow[r, chunk * 32:(chunk + 1) * 32, :].partition_broadcast(128)

    def row_bc(ap_row):
        return ap_row[0, :].partition_broadcast(128)

    kb.begin()
    op = kb.op
    modT = kb.sb("modT", [128, 384]); identf = kb.sb("identf", [128, 128]); mr = kb.sb("mr", [128, 384])
    pm = kb.ps("pm", [128, 512])
    op("sync", lambda e: e.dma_start(out=modT[:], in_=modT_d), writes=["modT"], dma="a0")
    op("sync", lambda e: e.dma_start(out=identf[:], in_=W["identf"]), writes=["identf"], dma="a1")
    for k in range(3):
        op("tensor", lambda e, k=k: e.transpose(out=pm[:, k * 128:(k + 1) * 128], in_=modT[:, k * 128:(k + 1) * 128], identity=identf[:]),
           reads=["modT", "identf"], writes=["pm"])
    op("vector", lambda e: e.tensor_copy(out=mr[:], in_=pm[:, 0:384]), reads=["pm"], writes=["mr"])
    for k in range(3):
        op("sync", lambda e, k=k: e.dma_start(out=MODROW[k * 128:(k + 1) * 128, :], in_=mr[:, k * 128:(k + 1) * 128]), reads=["mr"], writes=["MODROW"], dma="a2")
    kb.end()

    kb.begin()
    op = kb.op
    yT = kb.sb("yT", [128, 48, T], BF16)
    pb = [[kb.sb("pb%d_%d" % (s_, br), [128, 16, 256], BF16) for br in range(3)] for s_ in range(2)]
    gsb = kb.sb("gsb", [128, 3, T], BF16); sig = kb.sb("sig", [128, 3, T])
    t0_ = kb.sb("t0", [128, 512]); t1_ = kb.sb("t1", [128, 512])
    mst = [kb.sb("mst%d" % i, [128, T], BF16) for i in range(2)]
    psb = [[kb.ps("psb%d_%d" % (s_, br), [128, 512]) for br in range(3)] for s_ in range(2)]
    for br in range(3):
        op("sync", lambda e, br=br: e.dma_start(out=yT[:, br * 16:(br + 1) * 16, :], in_=Y3[br * 2048:(br + 1) * 2048, :].rearrange("(kt p) t -> p kt t", p=128)),
           writes=["yT"], dma="y%d" % br)
    cnt = 0
    for pc in range(16):
        sl = pc % 2
        for br in range(3):
            op("gpsimd", lambda e, sl=sl, br=br, pc=pc: e.dma_start(
                out=pb[sl][br][:], in_=W["w_br"][br].rearrange("(kt p) n -> p kt n", p=128)[:, :, pc * 256:(pc + 1) * 256]),
               writes=["pb%d_%d" % (sl, br)], dma="pb%d_%d" % (sl, br))
        for ct2 in range(2):
            dt = pc * 2 + ct2
            for br in range(3):
                op("sync", lambda e, br=br, dt=dt: e.dma_start(out=gsb[:, br, :], in_=G_T[br * 4096 + dt * 128:br * 4096 + (dt + 1) * 128, :]),
                   writes=["gsb"], dma="g%d" % br)
            op("scalar", lambda e: e.activation(out=sig[:], in_=gsb[:], func=AF.Sigmoid), reads=["gsb"], writes=["sig"])
            ms = dt % 2
            for (c0, n) in CHUNKS:
                ps_ = cnt % 2
                cnt += 1
                for br in range(3):
                    def mm(e, sl=sl, br=br, ct2=ct2, c0=c0, n=n, ps_=ps_):
                        ins = None
                        for kt in range(16):
                            ins = e.matmul(psb[ps_][br][:, :n], lhsT=pb[sl][br][:, kt, ct2 * 128:(ct2 + 1) * 128], rhs=yT[:, br * 16 + kt, c0:c0 + n],
                                           start=(kt == 0), stop=(kt == 15))
                        return ins
                    op("tensor", mm, reads=["pb%d_%d" % (sl, br), "yT"], writes=["psb%d_%d" % (ps_, br)])
                rk = ["psb%d_%d" % (ps_, br) for br in range(3)]
                op("vector", lambda e, ps_=ps_, c0=c0, n=n: e.tensor_tensor(out=t0_[:, :n], in0=psb[ps_][0][:, :n], in1=sig[:, 0, c0:c0 + n], op=ALU.mult),
                   reads=[rk[0], "sig"], writes=["t0"])
                op("vector", lambda e, ps_=ps_, c0=c0, n=n: e.tensor_tensor(out=t1_[:, :n], in0=psb[ps_][1][:, :n], in1=sig[:, 1, c0:c0 + n], op=ALU.mult),
                   reads=[rk[1], "sig"], writes=["t1"])
                op("vector", lambda e, n=n: e.tensor_tensor(out=t0_[:, :n], in0=t0_[:, :n], in1=t1_[:, :n], op=ALU.add), reads=["t0", "t1"], writes=["t0"])
                op("vector", lambda e, ps_=ps_, c0=c0, n=n: e.tensor_tensor(out=t1_[:, :n], in0=psb[ps_][2][:, :n], in1=sig[:, 2, c0:c0 + n], op=ALU.mult),
                   reads=[rk[2], "sig", "t0"], writes=["t1"])
                op("vector", lambda e, n=n, c0=c0, ms=ms: e.tensor_tensor(out=mst[ms][:, c0:c0 + n], in0=t0_[:, :n], in1=t1_[:, :n], op=ALU.add),
                   reads=["t0", "t1"], writes=["mst%d" % ms])
            op("sync", lambda e, dt=dt, ms=ms: e.dma_start(out=M_T[dt * 128:(dt + 1) * 128, :], in_=mst[ms][:]), reads=["mst%d" % ms], writes=["M_T"], dma="mst%d" % ms)
    kb.end()

    kb.begin()
    op = kb.op
    mT = kb.sb("mT", [128, 32, T], BF16)
    pan = [kb.sb("pan%d" % i, [128, 32, 512], BF16) for i in range(2)]
    bout = kb.sb("bout", [128, D]); g1bc = [kb.sb("g1bc%d" % r, [128, D]) for r in range(2)]
    zs = [kb.sb("zs%d" % i, [128, 512]) for i in range(2)]
    psz = [kb.ps("psz%d" % i, [128, 512]) for i in range(3)]
    op("sync", lambda e: e.dma_start(out=mT[:], in_=M_T.rearrange("(kt p) t -> p kt t", p=128)), reads=["M_T"], writes=["mT"], dma="mT")
    op("sync", lambda e: e.dma_start(out=bout[:], in_=row_bc(W["b_out"])), writes=["bout"], dma="b0")
    for r in range(2):
        op("sync", lambda e, r=r: e.dma_start(out=g1bc[r][:].rearrange("p (i q) -> p i q", q=128), in_=modrow_bc(2, r)), reads=["MODROW"], writes=["g1bc%d" % r], dma="b%d" % (r + 1))
    wo_v = W["w_out"].rearrange("(kt p) n -> p kt n", p=128)
    cnt = 0
    for pc in range(8):
        sl = pc % 2
        for q in range(4):
            op("gpsimd", lambda e, sl=sl, q=q, pc=pc: e.dma_start(out=pan[sl][:, q * 8:(q + 1) * 8, :], in_=wo_v[:, q * 8:(q + 1) * 8, pc * 512:(pc + 1) * 512]),
               writes=["pan%d_%d" % (sl, q)], dma="pan%d" % sl)
        for tt, (t0, n) in enumerate(TTILES):
            r = 0 if tt < 8 else 1
            pp = cnt % 3
            zb = cnt % 2
            cnt += 1

            def mm(e, sl=sl, t0=t0, n=n, pp=pp):
                ins = None
                for kt in range(32):
                    ins = e.matmul(psz[pp][:n, :], lhsT=mT[:, kt, t0:t0 + n], rhs=pan[sl][:, kt, :], start=(kt == 0), stop=(kt == 31))
                return ins
            op("tensor", mm, reads=["pan%d_%d" % (sl, q_) for q_ in range(4)] + ["mT"], writes=["psz%d" % pp])
            op("vector", lambda e, pp=pp, zb=zb, n=n, pc=pc: e.tensor_tensor(out=zs[zb][:n, :], in0=psz[pp][:n, :], in1=bout[:n, pc * 512:(pc + 1) * 512], op=ALU.add),
               reads=["psz%d" % pp, "bout"], writes=["zs%d" % zb])
            op("vector", lambda e, zb=zb, n=n, pc=pc, r=r: e.tensor_tensor(out=zs[zb][:n, :], in0=zs[zb][:n, :], in1=g1bc[r][:n, pc * 512:(pc + 1) * 512], op=ALU.mult),
               reads=["zs%d" % zb, "g1bc%d" % r], writes=["zs%d" % zb])
            op("sync", lambda e, zb=zb, n=n, t0=t0, pc=pc: e.dma_start(out=Z[t0:t0 + n, pc * 512:(pc + 1) * 512], in_=zs[zb][:n, :]),
               reads=["zs%d" % zb], writes=["Z"], dma="zs%d" % zb)
    kb.end()

    kb.begin()
    op = kb.op
    A = kb.sb("A", [128, D]); Bt = kb.sb("B", [128, D])
    lng = kb.sb("lng", [128, D]); lnb = kb.sb("lnb", [128, D])
    h2f = kb.sb("h2f", [128, 32, 128]); h2b = kb.sb("h2b", [128, 32, 128], BF16)
    modT = kb.sb("modT", [128, 384]); ops2 = kb.sb("ops2", [128, 64])
    identf = kb.sb("identf", [128, 128])
    rw = kb.sb("rw", [128, 32, 32]); rbb = kb.sb("rbb", [128, 32])
    stats = kb.sb("stats", [128, 48]); mv = kb.sb("mv", [128, 4]); mv2 = kb.sb("mv2", [128, 4])
    lg = kb.sb("lg", [128, 32]); mx8 = kb.sb("mx8", [128, 8]); msk = kb.sb("msk", [128, 32]); ex = kb.sb("ex", [128, 32])
    sm = kb.sb("sm", [128, 4]); cT = kb.sb("cT", [32, 128])
    pst = [kb.ps("pst%d" % i, [128, 512]) for i in range(2)]
    psr = kb.ps("psr", [128, 512]); psc = kb.ps("psc", [128, 512])
    op("sync", lambda e: e.dma_start(out=modT[:], in_=modT_d), writes=["modT"], dma="a0")
    op("sync", lambda e: e.dma_start(out=identf[:], in_=W["identf"]), writes=["identf"], dma="a1")
    op("sync", lambda e: e.dma_start(out=lng[:], in_=row_bc(W["ln1_g"])), writes=["lng"], dma="a2")
    op("sync", lambda e: e.dma_start(out=lnb[:], in_=row_bc(W["ln1_b"])), writes=["lnb"], dma="a3")
    op("sync", lambda e: e.dma_start(out=rw[:], in_=W["router_w"].rearrange("(kt p) n -> p kt n", p=128)), writes=["rw"], dma="a4")
    op("sync", lambda e: e.dma_start(out=rbb[:], in_=row_bc(W["router_b"])), writes=["rbb"], dma="a5")
    op("vector", lambda e: e.tensor_scalar(out=ops2[:], in0=modT[:, 256:320], scalar1=1.0, scalar2=None, op0=ALU.add), reads=["modT"], writes=["ops2"])
    modT3 = modT[:].rearrange("p (k r) -> p k r", r=2)
    ops23 = ops2[:].rearrange("p (k r) -> p k r", r=2)
    H2v = H2T.rearrange("(dt p) t -> p dt t", p=128)
    for tt, (t0, n) in enumerate(TTILES):
        r = 0 if tt < 8 else 1
        op("sync", lambda e, t0=t0, n=n: e.dma_start(out=A[:n, :], in_=Z[t0:t0 + n, :]), reads=["Z"], writes=["A"], dma="A")
        op("sync", lambda e, t0=t0, n=n: e.dma_start(out=Bt[:n, :], in_=x_own[t0:t0 + n, :]), writes=["B"], dma="B")
        op("vector", lambda e, n=n: e.scalar_tensor_tensor(out=A[:n, :], in0=Bt[:n, :], scalar=float(ALPHA), in1=A[:n, :], op0=ALU.mult, op1=ALU.add),
           reads=["A", "B"], writes=["A"])
        ks = ln_stats(op, A, n, stats, mv, "A", "s1")
        op("vector", lambda e, n=n: e.tensor_scalar(out=A[:n, :], in0=A[:n, :], scalar1=mv[:n, 0:1], scalar2=mv[:n, 2:3], op0=ALU.subtract, op1=ALU.mult),
           reads=["A"] + ks, writes=["A"])
        op("vector", lambda e, n=n: e.tensor_tensor(out=A[:n, :], in0=A[:n, :], in1=lng[:n, :], op=ALU.mult), reads=["A", "lng"], writes=["A"])
        op("vector", lambda e, n=n: e.tensor_tensor(out=A[:n, :], in0=A[:n, :], in1=lnb[:n, :], op=ALU.add), reads=["A", "lnb"], writes=["A"])
        op("sync", lambda e, t0=t0, n=n: e.dma_start(out=X1[t0:t0 + n, :], in_=A[:n, :]), reads=["A"], writes=["X1"], dma="x1")
        ks2 = ln_stats(op, A, n, stats, mv2, "A", "s2")
        op("vector", lambda e, n=n: e.tensor_scalar(out=Bt[:n, :], in0=A[:n, :], scalar1=mv2[:n, 0:1], scalar2=mv2[:n, 2:3], op0=ALU.subtract, op1=ALU.mult),
           reads=["A", "B"] + ks2, writes=["B"])
        for g in range(8):
            ps_ = pst[g % 2]

            def tr(e, g=g, n=n, ps_=ps_):
                ins = None
                for j in range(4):
                    dt = g * 4 + j
                    ins = e.transpose(out=ps_[:, j * 128:j * 128 + n], in_=Bt[:n, dt * 128:(dt + 1) * 128], identity=identf[:n, :n])
                return ins
            op("tensor", tr, reads=["B", "identf"], writes=["pst%d" % (g % 2)])

            def ev(e, g=g, n=n, ps_=ps_, r=r):
                ins = None
                for j in range(4):
                    dt = g * 4 + j
                    ins = e.tensor_scalar(out=h2f[:, dt, :n], in0=ps_[:, j * 128:j * 128 + n], scalar1=ops23[:, 128 + dt - 128, r:r + 1],
                                          scalar2=modT3[:, 96 + dt, r:r + 1], op0=ALU.mult, op1=ALU.add)
                return ins
            op("vector", ev, reads=["pst%d" % (g % 2), "ops2", "modT"], writes=["h2f"])
        op("scalar", lambda e, n=n: e.activation(out=h2b[:, :, :n], in_=h2f[:, :, :n], func=AF.Copy), reads=["h2f"], writes=["h2b"])
        op("sync", lambda e, t0=t0, n=n: e.dma_start(out=H2v[:, :, t0:t0 + n], in_=h2b[:, :, :n]), reads=["h2b"], writes=["H2T"], dma="h2")

        def rmm(e, n=n):
            ins = None
            for dt in range(32):
                ins = e.matmul(psr[:n, 0:32], lhsT=h2f[:, dt, :n], rhs=rw[:, dt, :], start=(dt == 0), stop=(dt == 31))
            return ins
        op("tensor", rmm, reads=["h2f", "rw"], writes=["psr"])
        op("vector", lambda e, n=n: e.tensor_tensor(out=lg[:n, :], in0=psr[:n, 0:32], in1=rbb[:n, :], op=ALU.add), reads=["psr", "rbb"], writes=["lg"])
        op("vector", lambda e, n=n: e.max(out=mx8[:n, :], in_=lg[:n, :]), reads=["lg"], writes=["mx8"])
        op("vector", lambda e, n=n: e.tensor_scalar(out=msk[:n, :], in0=lg[:n, :], scalar1=mx8[:n, 3:4], scalar2=None, op0=ALU.is_ge), reads=["lg", "mx8"], writes=["msk"])
        op("vector", lambda e, n=n: e.tensor_scalar(out=sm[:n, 0:1], in0=mx8[:n, 0:1], scalar1=-1.0, scalar2=None, op0=ALU.mult), reads=["mx8"], writes=["sm0"])
        op("scalar", lambda e, n=n: e.activation(out=ex[:n, :], in_=lg[:n, :], func=AF.Exp, bias=sm[:n, 0:1]), reads=["lg", "sm0"], writes=["ex"])
        op("vector", lambda e, n=n: e.tensor_tensor(out=ex[:n, :], in0=ex[:n, :], in1=msk[:n, :], op=ALU.mult), reads=["ex", "msk"], writes=["ex"])
        op("vector", lambda e, n=n: e.reduce_sum(out=sm[:n, 1:2], in_=ex[:n, :], axis=AX_X), reads=["ex"], writes=["sm1"])
        op("vector", lambda e, n=n: e.reciprocal(out=sm[:n, 2:3], in_=sm[:n, 1:2]), reads=["sm1"], writes=["sm2"])
        op("vector", lambda e, n=n: e.tensor_scalar(out=ex[:n, :], in0=ex[:n, :], scalar1=sm[:n, 2:3], scalar2=None, op0=ALU.mult), reads=["ex", "sm2"], writes=["ex"])
        op("tensor", lambda e, n=n: e.transpose(out=psc[0:32, 0:n], in_=ex[:n, :], identity=identf[:n, :n]), reads=["ex", "identf"], writes=["psc"])
        op("vector", lambda e, n=n: e.tensor_copy(out=cT[:, :n], in_=psc[0:32, 0:n]), reads=["psc"], writes=["cT"])
        op("sync", lambda e, t0=t0, n=n: e.dma_start(out=COMBT[:, t0:t0 + n], in_=cT[:, :n]), reads=["cT"], writes=["COMBT"], dma="ct")
    kb.end()

    kb.begin()
    op = kb.op
    h2T = kb.sb("h2T", [128, 32, T], BF16)
    acts = kb.sb("acts", [128, 8, T], BF16)
    gp = [kb.sb("gp%d" % i, [128, 32, 128], BF16) for i in range(2)]
    up = [kb.sb("up%d" % i, [128, 32, 128], BF16) for i in range(2)]
    dp = [kb.sb("dp%d" % i, [128, 8, 512], BF16) for i in range(2)]
    cbc = [kb.sb("cbc%d" % i, [128, T]) for i in range(2)]
    bg = kb.sb("bg", [128, 128]); bu = kb.sb("bu", [128, 128])
    cst = kb.sb("cst", [128, 4])
    g1 = [kb.sb("g1_%d" % i, [128, 512]) for i in range(2)]
    sg = [kb.sb("sg_%d" % i, [128, 512]) for i in range(2)]
    u1 = [kb.sb("u1_%d" % i, [128, 512]) for i in range(2)]
    fp_ = [kb.sb("fp%d" % i, [128, 512]) for i in range(2)]
    combT = kb.sb("combT", [32, T]); bdn = kb.sb("bdn", [32, D])
    psg = [kb.ps("psg%d" % i, [128, 512]) for i in range(2)]
    psu = [kb.ps("psu%d" % i, [128, 512]) for i in range(2)]
    psd = [kb.ps("psd%d" % i, [128, 512]) for i in range(3)]
    op("sync", lambda e: e.dma_start(out=h2T[:], in_=H2T.rearrange("(kt p) t -> p kt t", p=128)), reads=["H2T"], writes=["h2T"], dma="h2T")
    op("sync", lambda e: e.dma_start(out=bg[:], in_=W["b_gateT"]), writes=["bg"], dma="c0")
    op("sync", lambda e: e.dma_start(out=bu[:], in_=W["b_upT"]), writes=["bu"], dma="c1")
    op("sync", lambda e: e.dma_start(out=combT[:], in_=COMBT), reads=["COMBT"], writes=["combT"], dma="c2")
    op("sync", lambda e: e.dma_start(out=bdn[:], in_=W["b_down"]), writes=["bdn"], dma="c3")
    op("vector", lambda e: e.memset(cst[:, 0:1], 7.0), writes=["cst0"])
    op("vector", lambda e: e.memset(cst[:, 1:2], -7.0), writes=["cst1"])
    op("vector", lambda e: e.memset(cst[:, 2:3], 1.0), writes=["cst2"])
    CK = ["cst0", "cst1", "cst2"]
    fcnt = 0
    for pc in range(8):
        for tt, (t0, n) in enumerate(TTILES):
            pp = fcnt % 3
            fb = fcnt % 2
            fcnt += 1
            op("tensor", lambda e, pp=pp, t0=t0, n=n, pc=pc: e.matmul(psd[pp][:n, :], lhsT=combT[:, t0:t0 + n], rhs=bdn[:, pc * 512:(pc + 1) * 512], start=True, stop=True),
               reads=["combT", "bdn"], writes=["psd%d" % pp])
            op("vector", lambda e, pp=pp, fb=fb, n=n: e.tensor_copy(out=fp_[fb][:n, :], in_=psd[pp][:n, :]), reads=["psd%d" % pp], writes=["fp%d" % fb])
            op("sync", lambda e, fb=fb, t0=t0, n=n, pc=pc: e.dma_start(out=F[t0:t0 + n, pc * 512:(pc + 1) * 512], in_=fp_[fb][:n, :]),
               reads=["fp%d" % fb], writes=["F%d_%d" % (tt, pc)], dma="fo%d" % fb)
    wg_v = W["w_gate"].rearrange("e (kt p) f -> e p kt f", p=128)
    wu_v = W["w_up"].rearrange("e (kt p) f -> e p kt f", p=128)
    wd_v = W["w_down"].rearrange("(g j) (ft p) n -> g p (j ft) n", j=2, p=128)
    pcnt = 0
    ccnt = 0
    for ex_ in range(32):
        cb_ = ex_ % 2
        op("sync", lambda e, ex_=ex_, cb_=cb_: e.dma_start(out=cbc[cb_][:], in_=COMBT[ex_, :].partition_broadcast(128)), reads=["COMBT"], writes=["cbc%d" % cb_], dma="cbc%d" % cb_)
        for ft in range(4):
            sl = pcnt % 2
            pcnt += 1
            for hq in range(2):
                op("gpsimd", lambda e, sl=sl, ex_=ex_, ft=ft, hq=hq: e.dma_start(out=gp[sl][:, hq * 16:(hq + 1) * 16, :], in_=wg_v[ex_, :, hq * 16:(hq + 1) * 16, ft * 128:(ft + 1) * 128]),
                   writes=["gp%d_%d" % (sl, hq)], dma="gp%d" % sl)
                op("gpsimd", lambda e, sl=sl, ex_=ex_, ft=ft, hq=hq: e.dma_start(out=up[sl][:, hq * 16:(hq + 1) * 16, :], in_=wu_v[ex_, :, hq * 16:(hq + 1) * 16, ft * 128:(ft + 1) * 128]),
                   writes=["up%d_%d" % (sl, hq)], dma="up%d" % sl)
            bi = ex_ * 4 + ft
            for (c0, n) in CHUNKS:
                b_ = ccnt % 2
                ccnt += 1

                def mmg(e, sl=sl, c0=c0, n=n, b_=b_):
                    ins = None
                    for kt in range(32):
                        ins = e.matmul(psg[b_][:, :n], lhsT=gp[sl][:, kt, :], rhs=h2T[:, kt, c0:c0 + n], start=(kt == 0), stop=(kt == 31))
                    return ins

                def mmu(e, sl=sl, c0=c0, n=n, b_=b_):
                    ins = None
                    for kt in range(32):
                        ins = e.matmul(psu[b_][:, :n], lhsT=up[sl][:, kt, :], rhs=h2T[:, kt, c0:c0 + n], start=(kt == 0), stop=(kt == 31))
                    return ins
                op("tensor", mmg, reads=["gp%d_0" % sl, "gp%d_1" % sl, "h2T"], writes=["psg%d" % b_])
                op("tensor", mmu, reads=["up%d_0" % sl, "up%d_1" % sl, "h2T"], writes=["psu%d" % b_])
                op("vector", lambda e, b_=b_, n=n, bi=bi: e.tensor_scalar(out=g1[b_][:, :n], in0=psg[b_][:, :n], scalar1=bg[:, bi:bi + 1], scalar2=cst[:, 0:1], op0=ALU.add, op1=ALU.min),
                   reads=["psg%d" % b_, "bg"] + CK, writes=["g1_%d" % b_])
                op("scalar", lambda e, b_=b_, n=n: e.activation(out=sg[b_][:, :n], in_=g1[b_][:, :n], func=AF.Sigmoid, scale=1.702), reads=["g1_%d" % b_], writes=["sg_%d" % b_])
                op("vector", lambda e, b_=b_, n=n, bi=bi: e.tensor_scalar(out=u1[b_][:, :n], in0=psu[b_][:, :n], scalar1=bu[:, bi:bi + 1], scalar2=cst[:, 0:1], op0=ALU.add, op1=ALU.min),
                   reads=["psu%d" % b_, "bu"] + CK, writes=["u1_%d" % b_])
                op("vector", lambda e, b_=b_, n=n: e.tensor_scalar(out=u1[b_][:, :n], in0=u1[b_][:, :n], scalar1=cst[:, 1:2], scalar2=cst[:, 2:3], op0=ALU.max, op1=ALU.add),
                   reads=["u1_%d" % b_] + CK, writes=["u1_%d" % b_])
                op("vector", lambda e, b_=b_, n=n: e.tensor_tensor(out=g1[b_][:, :n], in0=g1[b_][:, :n], in1=sg[b_][:, :n], op=ALU.mult), reads=["g1_%d" % b_, "sg_%d" % b_], writes=["g1_%d" % b_])
                op("vector", lambda e, b_=b_, n=n: e.tensor_tensor(out=g1[b_][:, :n], in0=g1[b_][:, :n], in1=u1[b_][:, :n], op=ALU.mult), reads=["g1_%d" % b_, "u1_%d" % b_], writes=["g1_%d" % b_])
                j = (ex_ % 2) * 4 + ft
                op("vector", lambda e, b_=b_, n=n, c0=c0, j=j, cb_=cb_: e.tensor_tensor(out=acts[:, j, c0:c0 + n], in0=g1[b_][:, :n], in1=cbc[cb_][:, c0:c0 + n], op=ALU.mult),
                   reads=["g1_%d" % b_, "cbc%d" % cb_], writes=["acts"])
        if ex_ % 2 == 1:
            grp = ex_ // 2
            for pc in range(8):
                ds_ = (grp * 8 + pc) % 2
                op("gpsimd", lambda e, ds_=ds_, grp=grp, pc=pc: e.dma_start(out=dp[ds_][:], in_=wd_v[grp, :, :, pc * 512:(pc + 1) * 512]), writes=["dp%d" % ds_], dma="dp%d" % ds_)
                for tt, (t0, n) in enumerate(TTILES):
                    pp = fcnt % 3
                    fb = fcnt % 2
                    fcnt += 1
                    fk = "F%d_%d" % (tt, pc)

                    def mmd(e, ds_=ds_, t0=t0, n=n, pp=pp):
                        ins = None
                        for j in range(8):
                            ins = e.matmul(psd[pp][:n, :], lhsT=acts[:, j, t0:t0 + n], rhs=dp[ds_][:, j, :], start=(j == 0), stop=(j == 7))
                        return ins
                    op("tensor", mmd, reads=["acts", "dp%d" % ds_], writes=["psd%d" % pp])
                    op("sync", lambda e, fb=fb, t0=t0, n=n, pc=pc: e.dma_start(out=fp_[fb][:n, :], in_=F[t0:t0 + n, pc * 512:(pc + 1) * 512]),
                       reads=[fk], writes=["fp%d" % fb], dma="fi%d" % fb)
                    op("vector", lambda e, pp=pp, fb=fb, n=n: e.tensor_tensor(out=fp_[fb][:n, :], in0=psd[pp][:n, :], in1=fp_[fb][:n, :], op=ALU.add),
                       reads=["psd%d" % pp, "fp%d" % fb], writes=["fp%d" % fb])
                    op("sync", lambda e, fb=fb, t0=t0, n=n, pc=pc: e.dma_start(out=F[t0:t0 + n, pc * 512:(pc + 1) * 512], in_=fp_[fb][:n, :]),
                       reads=["fp%d" % fb], writes=[fk], dma="fo%d" % fb)
    kb.end()

    kb.begin()
    op = kb.op
    A = kb.sb("A", [128, D]); Bt = kb.sb("B", [128, D])
    lng = kb.sb("lng", [128, D]); lnb = kb.sb("lnb", [128, D])
    g2bc = [kb.sb("g2bc%d" % r, [128, D]) for r in range(2)]
    stats = kb.sb("stats", [128, 48]); mv = kb.sb("mv", [128, 4])
    op("sync", lambda e: e.dma_start(out=lng[:], in_=row_bc(W["ln2_g"])), writes=["lng"], dma="a2")
    op("sync", lambda e: e.dma_start(out=lnb[:], in_=row_bc(W["ln2_b"])), writes=["lnb"], dma="a3")
    for r in range(2):
        op("sync", lambda e, r=r: e.dma_start(out=g2bc[r][:].rearrange("p (i q) -> p i q", q=128), in_=modrow_bc(5, r)), writes=["g2bc%d" % r], dma="b%d" % r)
    for tt, (t0, n) in enumerate(TTILES):
        r = 0 if tt < 8 else 1
        op("sync", lambda e, t0=t0, n=n: e.dma_start(out=A[:n, :], in_=F[t0:t0 + n, :]), writes=["A"], dma="A")
        op("sync", lambda e, t0=t0, n=n: e.dma_start(out=Bt[:n, :], in_=X1[t0:t0 + n, :]), writes=["B"], dma="B")
        op("vector", lambda e, n=n, r=r: e.tensor_tensor(out=A[:n, :], in0=A[:n, :], in1=g2bc[r][:n, :], op=ALU.mult), reads=["A", "g2bc%d" % r], writes=["A"])
        op("vector", lambda e, n=n: e.scalar_tensor_tensor(out=A[:n, :], in0=Bt[:n, :], scalar=float(ALPHA), in1=A[:n, :], op0=ALU.mult, op1=ALU.add),
           reads=["A", "B"], writes=["A"])
        ks = ln_stats(op, A, n, stats, mv, "A", "s1")
        op("vector", lambda e, n=n: e.tensor_scalar(out=A[:n, :], in0=A[:n, :], scalar1=mv[:n, 0:1], scalar2=mv[:n, 2:3], op0=ALU.subtract, op1=ALU.mult),
           reads=["A"] + ks, writes=["A"])
        op("vector", lambda e, n=n: e.tensor_tensor(out=A[:n, :], in0=A[:n, :], in1=lng[:n, :], op=ALU.mult), reads=["A", "lng"], writes=["A"])
        op("vector", lambda e, n=n: e.tensor_tensor(out=A[:n, :], in0=A[:n, :], in1=lnb[:n, :], op=ALU.add), reads=["A", "lnb"], writes=["A"])
        op("sync", lambda e, t0=t0, n=n: e.dma_start(out=X2[t0:t0 + n, :], in_=A[:n, :]), reads=["A"], writes=["X2"], dma="x2")
    kb.end()


def build_p34(nblk=2, dbg=False):
    kb = KB()
    kb.end()
    nc = kb.nc
    Y3 = kb.din("Y3", [nblk * 6144, T], BF16)
    G_T = kb.din("G_T", [nblk * 12288, T], BF16)
    x_own = kb.din("x", [nblk * T, D])
    modT_d = kb.din("modT", [128, 384])
    W = dict(identf=kb.din("identf", [128, 128]), w_br=kb.din("w_br", [3, 2048, D]), w_out=kb.din("w_out", [D, D]), b_out=kb.din("b_out", [1, D]),
             ln1_g=kb.din("ln1_g", [1, D]), ln1_b=kb.din("ln1_b", [1, D]), router_w=kb.din("router_w", [D, 32]), router_b=kb.din("router_b", [1, 32]),
             w_gate=kb.din("w_gate", [32, D, 512]), w_up=kb.din("w_up", [32, D, 512]), w_down=kb.din("w_down", [32, 512, D]),
             b_gateT=kb.din("b_gateT", [128, 128]), b_upT=kb.din("b_upT", [128, 128]), b_down=kb.din("b_down", [32, D]),
             ln2_g=kb.din("ln2_g", [1, D]), ln2_b=kb.din("ln2_b", [1, D]))
    X2 = kb.dout("X2", [nblk * T, D])
    kind = "ExternalOutput" if dbg else "Internal"
    def scr_t(name, shape, dt):
        return nc.dram_tensor(name, list(shape), dt, kind=kind).ap()
    scr = dict(M_T=scr_t("M_T", [D, T], BF16), Z=scr_t("Z", [T, D], F32), X1=scr_t("X1", [T, D], F32), H2T=scr_t("H2T", [D, T], BF16),
               COMBT=scr_t("COMBT", [32, T], F32), F=scr_t("F", [T, D], F32), MODROW=scr_t("MODROW", [384, 128], F32))
    for blk in range(nblk):
        p34_body(kb, Y3[blk * 6144:(blk + 1) * 6144, :], G_T[blk * 12288:(blk + 1) * 12288, :], x_own[blk * T:(blk + 1) * T, :], modT_d, W,
                 X2[blk * T:(blk + 1) * T, :], scr)
    return kb.nc


def fmT(v, r=None):
    return np.ascontiguousarray(v.reshape(-1, 128).T)

def pm_inmaps(inp):
    rows = np.stack([inp['c'][0], inp['c'][1], inp['c_ctx']], 0)
    cT = np.ascontiguousarray(rows.reshape(3, 32, 128).transpose(2, 1, 0)).reshape(128, 96)
    maps = []
    for i in range(8):
        w = np.ascontiguousarray(inp['w_ada'][:, :, i*3072:(i+1)*3072]).reshape(2 * D, 3072)
        bb = inp['b_ada'][:, i*3072:(i+1)*3072].reshape(2, 24, 128)
        b_adaT = np.ascontiguousarray(bb.transpose(2, 0, 1)).reshape(128, 48)
        maps.append(dict(cT=cT, w_ada=w, b_adaT=b_adaT))
    return maps

def pm_assemble(res):
    full = np.concatenate([np.asarray(r['modS']).reshape(128, 2, 24, 3) for r in res], 2)
    out = {}
    for l in range(2):
        for b in range(2):
            out[(l, b)] = np.ascontiguousarray(np.stack([full[:, l, :, b], full[:, l, :, 2]], -1)).reshape(128, 384)
    return out

def p1_inmaps(inp, l, x_own, modTs):
    maps = []
    ident = np.eye(128, dtype=np.float32)
    psw = swap_perms()
    for i in range(4):
        blks = (2 * i, 2 * i + 1)
        b = blks[0] // 4
        maps.append(dict(x=np.concatenate([x_own[k] for k in blks], 0), modT=modTs[(l, b)], w_in=inp['w_in'][l],
                         rope=np.concatenate([rope_tables(k % 4) for k in blks], 0), psw=psw, ident=ident))
    return maps

def p1_split(res, modTs, l):
    out = []
    for k in range(8):
        r = res[k // 2]
        o = k % 2
        out.append(dict(FM=np.asarray(r['FM'])[o*20992:(o+1)*20992], VA=np.asarray(r['VA'])[o*T:(o+1)*T], VD=np.asarray(r['VD'])[o*T:(o+1)*T],
                        modT=modTs[(l, k // 4)]))
    return out

def own_tokens(inp):
    xs = []
    for i in range(8):
        b, j = divmod(i, 4)
        xs.append(np.concatenate([inp['x'][b, j*1024:(j+1)*1024], inp['ctx'][b, j*64:(j+1)*64]], 0))
    return xs


def p2_params(inp, l, g):
    cw = inp['conv_w'][l][:, g*512:(g+1)*512]
    conv_w = np.ascontiguousarray(cw.reshape(4, 4, 128).transpose(2, 1, 0)).reshape(128, 16)
    conv_b = fmT(inp['conv_b'][l][g*512:(g+1)*512])
    def dirvec(a):
        s = a[:, g*512:(g+1)*512].reshape(2, 4, 128)
        return np.ascontiguousarray(s.transpose(2, 0, 1)).reshape(128, 8)
    lru_p = np.concatenate([dirvec(inp['lru_br'][l]), dirvec(inp['lru_bi'][l]), dirvec(inp['lru_lambda'][l])], 1)
    wr = inp['lru_wr'][l][:, 4*g:4*g+4].reshape(8, 128, 128)
    wi = inp['lru_wi'][l][:, 4*g:4*g+4].reshape(8, 128, 128)
    lru_w = np.ascontiguousarray(np.concatenate([wr, wi], 0))
    k = np.arange(128)[:, None]; q = np.arange(128)[None, :]
    masks = np.stack([(k >= q), (k <= q)], 0).astype(np.float32).astype(BF)
    sink = np.ascontiguousarray(np.broadcast_to(inp['wa_sink'][l][4*g:4*g+4][None], (128, 4)))
    lqk = np.stack([inp['da_lq1'][l], inp['da_lk1'][l], inp['da_lq2'][l], inp['da_lk2'][l]], 0)
    lqk = np.ascontiguousarray(np.broadcast_to(lqk[None], (128, 4, 64)))
    gn = np.ascontiguousarray(np.broadcast_to(inp['da_norm_g'][l][None], (128, 128)))
    return dict(conv_w=conv_w, conv_b=conv_b, lru_p=np.ascontiguousarray(lru_p), lru_w=lru_w, masks=masks,
                identb=np.eye(128, dtype=np.float32).astype(BF), sink=sink, lqk=lqk, gn=gn)

def p2_inmaps(inp, l, p1res):
    maps = []
    for i in range(8):
        b, g = divmod(i, 4)
        srcs = [p1res[4*b + j] for j in range(4)]
        def rows(r0, n):
            lat = np.concatenate([s['FM'][r0:r0+n, 0:1024] for s in srcs], 1)
            cx = np.concatenate([s['FM'][r0:r0+n, 1024:1088] for s in srcs], 1)
            return np.concatenate([lat, cx], 1)
        FMX = np.concatenate([rows(g*512, 512), rows(2048 + g*512, 512), rows(4608 + g*512, 512), rows(6656 + g*512, 512), rows(4096 + g*128, 128)], 0)
        def vrows(key, c0, n):
            lat = np.concatenate([s[key][0:1024, c0:c0+n] for s in srcs], 0)
            cx = np.concatenate([s[key][1024:1088, c0:c0+n] for s in srcs], 0)
            return np.concatenate([lat, cx], 0)
        VX = np.concatenate([vrows('VA', g*128, 128), vrows('VD', g*512, 512)], 1)
        m = dict(FMX=np.ascontiguousarray(FMX), VX=np.ascontiguousarray(VX))
        m.update(p2_params(inp, l, g))
        maps.append(m)
    return maps


def p34_weights(inp, l):
    bgT = np.ascontiguousarray(inp['exp_b_gate'][l].reshape(32, 4, 128).transpose(2, 0, 1)).reshape(128, 128)
    buT = np.ascontiguousarray(inp['exp_b_up'][l].reshape(32, 4, 128).transpose(2, 0, 1)).reshape(128, 128)
    return dict(identf=np.eye(128, dtype=np.float32), w_br=np.stack([inp['w_br_a'][l], inp['w_br_b'][l], inp['w_br_c'][l]], 0),
                w_out=inp['w_out'][l], b_out=inp['b_out'][l][None], ln1_g=inp['ln1_g'][l][None], ln1_b=inp['ln1_b'][l][None],
                router_w=inp['router_w'][l], router_b=inp['router_b'][l][None], w_gate=inp['exp_w_gate'][l], w_up=inp['exp_w_up'][l],
                w_down=inp['exp_w_down'][l], b_gateT=bgT, b_upT=buT, b_down=inp['exp_b_down'][l], ln2_g=inp['ln2_g'][l][None], ln2_b=inp['ln2_b'][l][None])

def p34_inmaps(inp, l, p1res, p2res, x_own, nblk=1):
    wts = p34_weights(inp, l)
    maps = []
    for i in range(8 // nblk):
        Y3s, Gs, xs = [], [], []
        for k in range(i * nblk, (i + 1) * nblk):
            b, j = divmod(k, 4)
            parts = []
            for br in range(3):
                for g in range(4):
                    Y = p2res[4*b + g]['Y_T']
                    parts.append(np.concatenate([Y[br*512:(br+1)*512, j*1024:(j+1)*1024], Y[br*512:(br+1)*512, 4096 + j*64:4096 + (j+1)*64]], 1))
            Y3s.append(np.concatenate(parts, 0)); Gs.append(p1res[k]['FM'][8704:]); xs.append(x_own[k])
        m = dict(Y3=np.ascontiguousarray(np.concatenate(Y3s, 0)), G_T=np.ascontiguousarray(np.concatenate(Gs, 0)),
                 x=np.ascontiguousarray(np.concatenate(xs, 0)), modT=p1res[i * nblk]['modT'])
        m.update(wts)
        maps.append(m)
    return maps

def p34_split(res, nblk=1):
    return [np.asarray(res[k // nblk]['X2'])[(k % nblk) * T:(k % nblk + 1) * T] for k in range(8)]


_PROGS = {}


def _prog(key, fn):
    if key not in _PROGS:
        _PROGS[key] = fn()
    return _PROGS[key]


def _run(nc, maps):
    res = _bu.run_bass_kernel_spmd(nc, maps, core_ids=list(range(len(maps))))
    return res.results


def kernel(**inp):
    inp = {k: np.asarray(v) for k, v in inp.items()}
    x_own = own_tokens(inp)
    modTs = pm_assemble(_run(_prog("pm", build_pm), pm_inmaps(inp)))
    for l in range(2):
        p1res = p1_split(_run(_prog("p1", build_p1), p1_inmaps(inp, l, x_own, modTs)), modTs, l)
        r2 = _run(_prog("p2_%d" % l, lambda l=l: build_p2(l)), p2_inmaps(inp, l, p1res))
        p2res = [dict(Y_T=np.asarray(r["Y_T"])) for r in r2]
        del r2
        x_own = p34_split(_run(_prog("p34", lambda: build_p34(1)), p34_inmaps(inp, l, p1res, p2res, x_own, 1)), 1)
        del p1res, p2res
    out = np.empty((2, 4096, 4096), np.float32)
    for i in range(8):
        b, j = divmod(i, 4)
        out[b, j * 1024:(j + 1) * 1024] = x_own[i][0:1024]
    return out
```
